# Optimizing a Trainium2 kernel written in Bass

```python
import jax, jax.numpy as jnp
from jax import lax
import numpy as np

D_MODEL = 2048
BATCH = 8
SEQ = 4096
DEPTH = 1
DEC_BATCH = 16
DEC_SEQ = 2048
PAST_LEN = 128

N_META = 16
GRID_W = 64
Q_BLOCK = 128
N_HEADS = 16
N_KV_HEADS = 4
HEAD_DIM = D_MODEL // N_HEADS
Q_PER_KV = N_HEADS // N_KV_HEADS
Q_DIM = N_HEADS * HEAD_DIM
KV_DIM = N_KV_HEADS * HEAD_DIM
ROPE_AXIS_DIM = HEAD_DIM // 2
ROPE_HALF = ROPE_AXIS_DIM // 2
ROPE_THETA = 10000.0
N_FOURIER_GROUPS = 4
FOURIER_GROUP_DIM = D_MODEL // 8
FOURIER_DIM = N_FOURIER_GROUPS * FOURIER_GROUP_DIM
IN_DIM = FOURIER_DIM + Q_DIM + 2 * KV_DIM + 2 * D_MODEL
IN_SPLITS = (FOURIER_DIM, FOURIER_DIM + Q_DIM, FOURIER_DIM + Q_DIM + KV_DIM,
             FOURIER_DIM + Q_DIM + 2 * KV_DIM, FOURIER_DIM + Q_DIM + 2 * KV_DIM + D_MODEL)
N_EXPERT_GROUPS = 4
EXPERTS_PER_GROUP = 8
N_EXPERTS = N_EXPERT_GROUPS * EXPERTS_PER_GROUP
TOP_K = 2
D_EXPERT = D_MODEL // 2
MOE_BLOCK = 128
NORM_EPS = 1e-6

kernel_name = 'hybrid_gqa_fnet_hmoe_encoder'


def rms_norm(x, g):
    xf = x.astype(jnp.float32)
    y = xf * lax.rsqrt(jnp.mean(xf * xf, axis=-1, keepdims=True) + NORM_EPS)
    return (y * g.astype(jnp.float32)).astype(x.dtype)


def axial_rope_tables(n_tokens):
    rows = n_tokens // GRID_W
    row = jnp.repeat(jnp.arange(rows, dtype=jnp.float32), GRID_W)
    col = jnp.tile(jnp.arange(GRID_W, dtype=jnp.float32), rows)
    inv_freq = ROPE_THETA ** (-jnp.arange(ROPE_HALF, dtype=jnp.float32) / ROPE_HALF)
    ang = jnp.stack([row[:, None] * inv_freq, col[:, None] * inv_freq], axis=1)
    ang = jnp.concatenate([jnp.zeros((N_META, 2, ROPE_HALF), jnp.float32), ang], axis=0)
    return jnp.cos(ang), jnp.sin(ang)


def apply_axial_rope(x, cos, sin):
    B, L, H, _ = x.shape
    xs = x.astype(jnp.float32).reshape(B, L, H, 2, 2, ROPE_HALF)
    x1, x2 = xs[..., 0, :], xs[..., 1, :]
    c, s = cos[:, None], sin[:, None]
    out = jnp.stack([x1 * c - x2 * s, x1 * s + x2 * c], axis=-2)
    return out.reshape(B, L, H, HEAD_DIM).astype(x.dtype)


def gqa_attention(q, k, v):
    B, L, _, _ = q.shape
    n_real = L - N_META
    n_blk = n_real // Q_BLOCK
    scale = HEAD_DIM ** -0.5
    qg = q.reshape(B, L, N_KV_HEADS, Q_PER_KV, HEAD_DIM)

    def attend(qb):
        s = jnp.einsum('bqkgd,bskd->bkgqs', qb, k).astype(jnp.float32) * scale
        p = jax.nn.softmax(s, axis=-1).astype(v.dtype)
        return jnp.einsum('bkgqs,bskd->bqkgd', p, v)

    meta_out = attend(qg[:, :N_META]).reshape(B, N_META, Q_DIM)
    q_blocks = qg[:, N_META:].reshape(B, n_blk, Q_BLOCK, N_KV_HEADS, Q_PER_KV, HEAD_DIM)
    q_blocks = jnp.moveaxis(q_blocks, 1, 0)
    real_out = lax.map(attend, q_blocks)
    real_out = jnp.moveaxis(real_out, 0, 1).reshape(B, n_real, Q_DIM)
    return jnp.concatenate([meta_out, real_out], axis=1)


def fourier_mix(u):
    B, L, _ = u.shape
    ug = u.astype(jnp.float32).reshape(B, L, N_FOURIER_GROUPS, FOURIER_GROUP_DIM)
    z = jnp.fft.fftn(ug, axes=(1, 3), norm='ortho').real
    return z.reshape(B, L, FOURIER_DIM).astype(u.dtype)


def token_mixer(h, cos, sin, w_in, q_gain, k_gain, w_attn_o, w_fourier_o, w_out):
    B, L, _ = h.shape
    proj = h @ w_in
    u_f, q, k, v, g_a, g_f = jnp.split(proj, IN_SPLITS, axis=-1)
    q = apply_axial_rope(rms_norm(q.reshape(B, L, N_HEADS, HEAD_DIM), q_gain), cos, sin)
    k = apply_axial_rope(rms_norm(k.reshape(B, L, N_KV_HEADS, HEAD_DIM), k_gain), cos, sin)
    v = v.reshape(B, L, N_KV_HEADS, HEAD_DIM)
    a_branch = gqa_attention(q, k, v) @ w_attn_o
    f_branch = fourier_mix(u_f) @ w_fourier_o
    merged = jax.nn.sigmoid(g_a) * a_branch + jax.nn.sigmoid(g_f) * f_branch
    return merged @ w_out


def hierarchical_moe(x, w_rg, w_re, w_gate, w_up, w_down):
    T, D = x.shape
    p_group = jax.nn.softmax((x @ w_rg).astype(jnp.float32), axis=-1)
    p_g, g_idx = lax.top_k(p_group, 1)
    logits_e = (x @ w_re).astype(jnp.float32).reshape(T, N_EXPERT_GROUPS, EXPERTS_PER_GROUP)
    logits_in = jnp.take_along_axis(logits_e, g_idx[:, :, None], axis=1)[:, 0]
    p_e, e_local = lax.top_k(jax.nn.softmax(logits_in, axis=-1), TOP_K)
    p_e = p_e / jnp.sum(p_e, axis=-1, keepdims=True)
    combine_w = (p_g * p_e).astype(x.dtype)
    expert_id = (g_idx * EXPERTS_PER_GROUP + e_local).astype(jnp.int32)

    A = T * TOP_K
    flat_e = expert_id.reshape(A)
    order = jnp.argsort(flat_e).astype(jnp.int32)
    sorted_e = flat_e[order]
    counts = jnp.bincount(flat_e, length=N_EXPERTS).astype(jnp.int32)
    start = jnp.cumsum(counts) - counts
    padded = (counts + MOE_BLOCK - 1) // MOE_BLOCK * MOE_BLOCK
    pad_end = jnp.cumsum(padded)
    pad_start = pad_end - padded
    dest = (pad_start[sorted_e] + jnp.arange(A, dtype=jnp.int32) - start[sorted_e]).astype(jnp.int32)
    n_blocks = -(-A // MOE_BLOCK) + N_EXPERTS
    P = n_blocks * MOE_BLOCK
    token_of_slot = jnp.full((P,), T, jnp.int32).at[dest].set(order // TOP_K)
    block_expert = jnp.minimum(
        jnp.searchsorted(pad_end, jnp.arange(n_blocks, dtype=jnp.int32) * MOE_BLOCK, side='right'),
        N_EXPERTS - 1)
    x_pad = jnp.concatenate([x, jnp.zeros((1, D), x.dtype)], axis=0)
    xb = x_pad[token_of_slot].reshape(n_blocks, MOE_BLOCK, D)

    def run_block(args):
        xs, e = args
        hid = jax.nn.silu(xs @ w_gate[e]) * (xs @ w_up[e])
        return hid @ w_down[e]

    yb = lax.map(run_block, (xb, block_expert)).reshape(P, D)
    slot_of_assign = jnp.zeros((A,), jnp.int32).at[order].set(dest).reshape(T, TOP_K)
    return jnp.einsum('tkd,tk->td', yb[slot_of_assign], combine_w)


def encoder_forward(x, meta_tokens, mix_norm, w_in, q_gain, k_gain, w_attn_o, w_fourier_o, w_out,
                    moe_norm, w_router_group, w_router_expert, w_expert_gate, w_expert_up,
                    w_expert_down, final_norm):
    B, N, D = x.shape
    meta = jnp.broadcast_to(meta_tokens[None].astype(x.dtype), (B, N_META, D))
    h = jnp.concatenate([meta, x], axis=1)
    L = h.shape[1]
    cos, sin = axial_rope_tables(N)
    for l in range(DEPTH):
        h = h + token_mixer(rms_norm(h, mix_norm[l]), cos, sin, w_in[l], q_gain[l], k_gain[l],
                            w_attn_o[l], w_fourier_o[l], w_out[l])
        hn = rms_norm(h, moe_norm[l]).reshape(B * L, D)
        h = h + hierarchical_moe(hn, w_router_group[l], w_router_expert[l], w_expert_gate[l],
                                 w_expert_up[l], w_expert_down[l]).reshape(B, L, D)
    return rms_norm(h, final_norm)[:, N_META:]


def setup_inputs(seed: int = 0) -> dict:
    key = jax.random.key(seed)
    ks = jax.random.split(key, 20)
    f32 = jnp.float32

    def nrm(k, shape, scale):
        return jax.random.normal(k, shape, f32) * scale

    def gain(k, shape):
        return 1.0 + 0.02 * jax.random.normal(k, shape, f32)

    return {
        'x_prompt': nrm(ks[0], (BATCH, SEQ, D_MODEL), 1.0),
        'x_sample': nrm(ks[1], (DEC_BATCH, DEC_SEQ, D_MODEL), 1.0),
        'meta_tokens': nrm(ks[2], (N_META, D_MODEL), 1.0),
        'mix_norm': gain(ks[3], (DEPTH, D_MODEL)),
        'w_in': nrm(ks[4], (DEPTH, D_MODEL, IN_DIM), D_MODEL ** -0.5),
        'q_gain': gain(ks[5], (DEPTH, HEAD_DIM)),
        'k_gain': gain(ks[6], (DEPTH, HEAD_DIM)),
        'w_attn_o': nrm(ks[7], (DEPTH, Q_DIM, D_MODEL), Q_DIM ** -0.5),
        'w_fourier_o': nrm(ks[8], (DEPTH, FOURIER_DIM, D_MODEL), FOURIER_DIM ** -0.5),
        'w_out': nrm(ks[9], (DEPTH, D_MODEL, D_MODEL), D_MODEL ** -0.5),
        'moe_norm': gain(ks[10], (DEPTH, D_MODEL)),
        'w_router_group': nrm(ks[11], (DEPTH, D_MODEL, N_EXPERT_GROUPS), D_MODEL ** -0.5),
        'w_router_expert': nrm(ks[12], (DEPTH, D_MODEL, N_EXPERTS), D_MODEL ** -0.5),
        'w_expert_gate': nrm(ks[13], (DEPTH, N_EXPERTS, D_MODEL, D_EXPERT), D_MODEL ** -0.5),
        'w_expert_up': nrm(ks[14], (DEPTH, N_EXPERTS, D_MODEL, D_EXPERT), D_MODEL ** -0.5),
        'w_expert_down': nrm(ks[15], (DEPTH, N_EXPERTS, D_EXPERT, D_MODEL), D_EXPERT ** -0.5),
        'final_norm': gain(ks[16], (D_MODEL,)),
    }


def reference(x_prompt, x_sample, meta_tokens, mix_norm, w_in, q_gain, k_gain, w_attn_o,
              w_fourier_o, w_out, moe_norm, w_router_group, w_router_expert, w_expert_gate,
              w_expert_up, w_expert_down, final_norm):
    y_prompt = encoder_forward(x_prompt, meta_tokens, mix_norm, w_in, q_gain, k_gain, w_attn_o,
                               w_fourier_o, w_out, moe_norm, w_router_group, w_router_expert,
                               w_expert_gate, w_expert_up, w_expert_down, final_norm)
    y_sample = encoder_forward(x_sample, meta_tokens, mix_norm, w_in, q_gain, k_gain, w_attn_o,
                               w_fourier_o, w_out, moe_norm, w_router_group, w_router_expert,
                               w_expert_gate, w_expert_up, w_expert_down, final_norm)
    return (y_prompt, y_sample)
```

```python
import numpy as np
import ml_dtypes
from contextlib import ExitStack
import concourse.bass as bass
import concourse.mybir as mybir
from concourse.bass_utils import run_bass_kernel_spmd

F32 = mybir.dt.float32
BF16 = mybir.dt.bfloat16
I32 = mybir.dt.int32
AF = mybir.ActivationFunctionType
ALU = mybir.AluOpType
AX = mybir.AxisListType

D = 2048
NH = 16
NKV = 4
HD = 128
FD = 1024
DE = 1024
NE = 32
NMETA = 16
GRID_W = 64
EPS = 1e-6
CAP = 640
NSLOT = NE * CAP


class Sem:
    def __init__(self, h, name):
        self.h = h
        self.name = name
        self.count = 0


class Buf:
    def __init__(self, name, acc=False):
        self.name = name
        self.w = {}
        self.r = {}
        self.acc = acc
        self.dsem = None


class EngW:
    def __init__(self, eng, sem, name):
        self.eng = eng
        self.sem = sem
        self.name = name
        self.seen = {}


class Ctx:
    def __init__(self, nc, es):
        self.nc = nc
        self.es = es
        self.uid = 0
        self.pool = []
        for i in range(88):
            h = es.enter_context(nc.semaphore(f"s{i}"))
            self.pool.append(Sem(h, f"s{i}"))
        self.allsems = list(self.pool)
        self.PE = EngW(nc.tensor, self.pool.pop(), "PE")
        self.ACT = EngW(nc.scalar, self.pool.pop(), "ACT")
        self.DVE = EngW(nc.vector, self.pool.pop(), "DVE")
        self.POOL = EngW(nc.gpsimd, self.pool.pop(), "POOL")
        self.SP = EngW(nc.sync, self.pool.pop(), "SP")
        self.engs = [self.PE, self.ACT, self.DVE, self.POOL, self.SP]

    def name(self, s):
        self.uid += 1
        return f"{s}_{self.uid}"

    def _need(self, reads, writes):
        need = {}

        def add(tok):
            k = id(tok[0])
            if k not in need or need[k][1] < tok[1]:
                need[k] = tok
        for b in reads:
            for t in b.w.values():
                add(t)
        for b in writes:
            for t in b.w.values():
                add(t)
            for t in b.r.values():
                add(t)
        return need

    def _wait(self, E, need):
        for k, (sem, val) in need.items():
            if sem is E.sem and E is self.PE:
                continue
            if E.seen.get(k, 0) >= val:
                continue
            E.eng.wait_ge(sem.h, val)
            E.seen[k] = val

    def _commit(self, tok, reads, writes):
        k = id(tok[0])
        for b in reads:
            b.r[k] = tok
        for b in writes:
            if b.acc:
                b.w[k] = tok
            else:
                b.w = {k: tok}
                b.r = {}

    def begin(self, E, reads=(), writes=()):
        self._wait(E, self._need(reads, writes))

    def end(self, E, ins, reads=(), writes=()):
        E.sem.count += 1
        ins.then_inc(E.sem.h, 1)
        self._commit((E.sem, E.sem.count), reads, writes)

    def op(self, E, fn, reads=(), writes=()):
        self.begin(E, reads, writes)
        ins = fn(E.eng)
        self.end(E, ins, reads, writes)

    def dma(self, E, fn, sb, reads=(), writes=()):
        if sb.dsem is None:
            sb.dsem = self.pool.pop()
        self.begin(E, reads, writes)
        ins = fn(E.eng)
        sb.dsem.count += 16
        ins.then_inc(sb.dsem.h, 16)
        self._commit((sb.dsem, sb.dsem.count), reads, writes)

    def release(self, bufs):
        for b in bufs:
            if b.dsem is not None:
                self.pool.append(b.dsem)
                b.dsem = None

    def barrier(self):
        for E in self.engs:
            need = {}
            for s in self.allsems:
                if s.count > 0:
                    need[id(s)] = (s, s.count)
            for k, (sem, val) in need.items():
                if E.seen.get(k, 0) >= val:
                    continue
                E.eng.wait_ge(sem.h, val)
                E.seen[k] = val


class Scope:
    def __init__(self, ctx):
        self.ctx = ctx
        self.es = ExitStack()
        self.bufs = []

    def __enter__(self):
        self.es.__enter__()
        return self

    def __exit__(self, *a):
        self.ctx.barrier()
        self.ctx.release(self.bufs)
        return self.es.__exit__(*a)

    def sb(self, name, shape, dt):
        t = self.es.enter_context(self.ctx.nc.sbuf_tensor(self.ctx.name(name), shape, dt))
        b = Buf(name)
        self.bufs.append(b)
        return t, b

    def ps(self, name, shape, dt):
        t = self.es.enter_context(self.ctx.nc.psum_tensor(self.ctx.name(name), shape, dt))
        b = Buf(name)
        self.bufs.append(b)
        return t, b

    def ring(self, name, n, shape, dt, psum=False):
        return Ring([(self.ps if psum else self.sb)(f"{name}{i}", shape, dt) for i in range(n)])


class Ring:
    def __init__(self, items):
        self.items = items
        self.i = 0

    def next(self):
        it = self.items[self.i % len(self.items)]
        self.i += 1
        return it


def _bf(a):
    return np.ascontiguousarray(a.astype(ml_dtypes.bfloat16))


def rope_tables(n):
    half = 32
    inv = (10000.0 ** (-np.arange(half, dtype=np.float32) / half)).astype(np.float32)
    t = np.arange(n)
    row = (t // GRID_W).astype(np.float32)
    col = (t % GRID_W).astype(np.float32)
    i = np.arange(HD)
    axis = i // 64
    f = i % 32
    pos = np.where(axis[:, None] == 0, row[None, :], col[None, :]).astype(np.float32)
    ang = (pos * inv[f][:, None]).astype(np.float32)
    return np.cos(ang).astype(np.float32), np.sin(ang).astype(np.float32)


def rot_matrix_T():
    R = np.zeros((HD, HD), np.float32)
    for i in range(HD):
        if (i % 64) < 32:
            R[i, i + 32] = -1.0
        else:
            R[i, i - 32] = 1.0
    return R.T.copy()


def chan_dft_table():
    c = np.arange(256)[:, None].astype(np.float64)
    cp = np.arange(256)[None, :].astype(np.float64)
    b = 2 * np.pi * ((c * cp) % 256) / 256.0
    tab = np.concatenate([np.cos(b) / 16.0, np.sin(b) / 16.0], axis=1)
    return tab.reshape(2, 128, 512).transpose(1, 0, 2).copy()


def pos_dft_table(n, lb):
    L = n + NMETA
    nt = 1 + n // 128
    lidx = np.zeros((128, nt), np.int64)
    valid = np.zeros((128, nt), bool)
    lidx[:16, 0] = np.arange(16)
    valid[:16, 0] = True
    for t in range(1, nt):
        lidx[:, t] = 16 + (t - 1) * 128 + np.arange(128)
        valid[:, t] = True
    out = np.zeros((n // lb, 128, nt, 2, lb), ml_dtypes.bfloat16)
    s = 1.0 / np.sqrt(L)
    for j in range(n // lb):
        lp = 16 + j * lb + np.arange(lb)
        a = 2 * np.pi * ((lidx[:, :, None] * lp[None, None, :]) % L) / L
        c = np.cos(a) * s * valid[:, :, None]
        sn = -np.sin(a) * s * valid[:, :, None]
        out[j, :, :, 0, :] = c.astype(ml_dtypes.bfloat16)
        out[j, :, :, 1, :] = sn.astype(ml_dtypes.bfloat16)
    return out


_CONST_CACHE = {}


def host_consts(seq_ns):
    key = tuple(seq_ns)
    if key in _CONST_CACHE:
        return _CONST_CACHE[key]
    c = {}
    c["ident_b"] = _bf(np.eye(128, dtype=np.float32))
    c["ident_f"] = np.eye(128, dtype=np.float32)
    c["ones_b"] = _bf(np.ones((128, 128), np.float32))
    c["onesm_b"] = _bf(np.full((128, 128), 1.0 / 128, np.float32))
    c["ones_f"] = np.ones((128, 128), np.float32)
    tri = (np.arange(128)[:, None] < np.arange(128)[None, :]).astype(np.float32)
    c["tri_f"] = tri
    c["rotT_b"] = _bf(rot_matrix_T())
    c["cc_b"] = _bf(chan_dft_table())
    c["iota_e"] = np.tile(np.arange(NE, dtype=np.float32)[None, :] * CAP, (128, 1))
    for n in sorted(set(seq_ns)):
        cs, sn = rope_tables(n)
        c[f"ropec_{n}"] = cs
        c[f"ropes_{n}"] = sn
        c[f"dft_{n}"] = pos_dft_table(n, lb_for(n))
    _CONST_CACHE[key] = c
    return c


def lb_for(n):
    return 256 if n > 2048 else min(512, n)


def gb_for(n):
    return 2 if n > 2048 else 4


class Prog:
    pass


def build_program(seq_ns, debug=False, upto=99):
    nc = bass.Bass("TRN2", target_bir_lowering=False)
    G = Prog()
    G.nc = nc
    G.seq_ns = list(seq_ns)
    G.debug = debug
    G.din = {}
    G.dout = {}
    G.scr = {}
    G.dbuf = {}

    def din(name, shape, dt):
        G.din[name] = nc.dram_tensor(name, list(shape), dt, kind="ExternalInput").ap()
        return G.din[name]

    def dscr(name, shape, dt, dbg=True):
        kind = "ExternalOutput" if (debug and dbg) else "Internal"
        G.scr[name] = nc.dram_tensor(name, list(shape), dt, kind=kind).ap()
        G.dbuf[name] = Buf(name, acc=True)
        return G.scr[name]

    for i, n in enumerate(seq_ns):
        din(f"x{i}", [n, D], F32)
        G.dout[f"y{i}"] = nc.dram_tensor(f"y{i}", [n, D], F32, kind="ExternalOutput").ap()
        G.dbuf[f"y{i}"] = Buf(f"y{i}", acc=True)
    din("meta", [NMETA, D], F32)
    din("g1b", [128, D], F32)
    din("g2b", [128, D], F32)
    din("gfb", [128, D], F32)
    din("qg", [128, 1], F32)
    din("kg", [128, 1], F32)
    din("w_in", [D, 8192], F32)
    if upto >= 4:
        din("w_ao", [D, D], F32)
        din("w_fo", [FD, D], F32)
    if upto >= 5:
        din("w_out", [D, D], F32)
        din("w_r", [D, 36], F32)
    if upto >= 6:
        din("w_eg", [NE, D, DE], F32)
        din("w_eu", [NE, D, DE], F32)
        din("w_ed", [NE, DE, D], F32)
    din("ident_b", [128, 128], BF16)
    din("ident_f", [128, 128], F32)
    din("ones_b", [128, 128], BF16)
    din("onesm_b", [128, 128], BF16)
    din("ones_f", [128, 128], F32)
    din("tri_f", [128, 128], F32)
    din("rotT_b", [128, 128], BF16)
    din("cc_b", [128, 2, 512], BF16)
    din("iota_e", [128, NE], F32)
    for n in sorted(set(seq_ns)):
        din(f"ropec_{n}", [128, n], F32)
        din(f"ropes_{n}", [128, n], F32)
        lb = lb_for(n)
        din(f"dft_{n}", [n // lb, 128, 1 + n // 128, 2, lb], BF16)

    dscr("kTm", [NKV, 128, NMETA], BF16)
    dscr("vm", [NMETA, 512], BF16)
    dscr("ABm", [NMETA, 2048], BF16)
    for i, n in enumerate(seq_ns):
        dscr(f"qT{i}", [NH, 128, n], BF16)
        dscr(f"kT{i}", [NKV, 128, n], BF16)
        dscr(f"v{i}", [n, 512], BF16)
        dscr(f"AB{i}", [n, 2048], BF16)
        dscr(f"sga{i}", [D, n], BF16)
        dscr(f"sgf{i}", [D, n], BF16)
        dscr(f"ZT{i}", [FD, n], BF16)
        dscr(f"attnT{i}", [D, n], BF16)
        dscr(f"mT{i}", [D, n], BF16)
        dscr(f"h1_{i}", [n, D], F32)
    dscr("Xs", [NSLOT, D], BF16, dbg=False)
    dscr("Ys", [NSLOT, D], F32, dbg=False)

    with ExitStack() as es:
        ctx = Ctx(nc, es)
        G.ctx = ctx
        G.bcreg = es.enter_context(nc.gpsimd.register("bcreg"))
        nc.gpsimd.reg_mov(G.bcreg, NSLOT - 1)
        with Scope(ctx) as CS:
            G.CS = CS
            load_consts(G)
            NT = sum(seq_ns) // 128
            G.cw1, G.cwb = CS.sb("cw1", [128, NT], F32)
            G.cw2, _ = CS.sb("cw2", [128, NT], F32)
            G.sl1, G.slb = CS.sb("sl1", [128, NT], I32)
            G.sl2, _ = CS.sb("sl2", [128, NT], I32)
            precast(G)
            G.marks = []
            mark = lambda nm: G.marks.append((nm, ctx.PE.sem.count, ctx.ACT.sem.count, ctx.DVE.sem.count))
            mark("A")
            if upto >= 1:
                phase_A(G)
            if upto >= 2:
                for i in range(len(seq_ns)):
                    mark(f"F{i}")
                    phase_F(G, i)
            if upto >= 3:
                for i in range(len(seq_ns)):
                    mark(f"At{i}")
                    phase_At(G, i)
            if upto >= 4:
                mark("G1")
                phase_G1(G)
            if upto >= 5:
                mark("G2")
                phase_G2(G)
            if upto >= 6:
                mark("M2")
                phase_M2(G)
            if upto >= 7:
                mark("M3")
                phase_M3(G)
            mark("end")
            ctx.barrier()
    return nc, G


def precast(G):
    nc, ctx = G.nc, G.ctx
    POOL = ctx.POOL
    G.wb = {}
    G.wbb = {}

    def one(name, src, rows, cols, upto_ok):
        if not upto_ok:
            return
        dst = nc.dram_tensor(name + "_bf", [rows, cols], BF16).ap()
        G.wb[name] = dst
        nblk = cols // 512
        grp = 4
        for g0 in range(0, nblk, grp):
            holder = Buf(f"{name}_h{g0}")
            G.CS.bufs.append(holder)
            db = Buf(f"{name}_d{g0}", acc=True)
            for b in range(g0, min(nblk, g0 + grp)):
                G.wbb[(name, b)] = db
                ctx.dma(POOL, lambda e: e.dma_start(out=dst[:, b * 512:(b + 1) * 512],
                                                    in_=src[:, b * 512:(b + 1) * 512]), sb=holder, writes=[db])
    one("w_in", G.din["w_in"], D, 8192, True)
    one("w_ao", G.din.get("w_ao"), D, D, "w_ao" in G.din)
    one("w_fo", G.din.get("w_fo"), FD, D, "w_fo" in G.din)
    one("w_out", G.din.get("w_out"), D, D, "w_out" in G.din)


def load_consts(G):
    ctx, CS = G.ctx, G.CS
    G.c = {}
    G.cb = {}
    for name, shape, dt in [("ident_b", [128, 128], BF16), ("ident_f", [128, 128], F32),
                            ("ones_b", [128, 128], BF16), ("onesm_b", [128, 128], BF16),
                            ("ones_f", [128, 128], F32), ("tri_f", [128, 128], F32),
                            ("rotT_b", [128, 128], BF16), ("cc_b", [128, 2, 512], BF16),
                            ("iota_e", [128, NE], F32), ("qg", [128, 1], F32), ("kg", [128, 1], F32)]:
        t, b = CS.sb(name, shape, dt)
        G.c[name], G.cb[name] = t, b
        src = G.din[name]
        ctx.dma(ctx.SP, lambda e, t=t, src=src: e.dma_start(out=t[:], in_=src), sb=b, writes=[b])


def phase_A(G):
    nc, ctx = G.nc, G.ctx
    PE, ACT, DVE, POOL, SP = ctx.PE, ctx.ACT, ctx.DVE, ctx.POOL, ctx.SP
    c, cb = G.c, G.cb
    SBW = min(1024, min(G.seq_ns))
    w_in_r = G.wb["w_in"].rearrange("(c p) n -> p c n", p=128)
    with Scope(ctx) as S:
        ntmax = SBW // 128
        hn_slots = []
        for i in range(2):
            t, _ = S.sb(f"hnT{i}", [128, 16, SBW], BF16)
            hn_slots.append((t, [Buf(f"hnT{i}_{j}") for j in range(ntmax)]))
        uT, _ = S.sb("uT", [128, 8, SBW], BF16)
        uTb = [Buf(f"uT{i}") for i in range(max(1, SBW // 512))]
        wring = S.ring("w", 3, [128, 16, 512], BF16)
        xring = S.ring("x", 2, [128, D], F32)
        xnring = S.ring("xn", 2, [128, D], BF16)
        g1b, g1bb = S.sb("g1b", [128, D], F32)
        ctx.dma(SP, lambda e: e.dma_start(out=g1b[:], in_=G.din["g1b"]), sb=g1bb, writes=[g1bb])
        ropeCr = S.ring("ropeC", 1, [128, SBW], F32)
        ropeSr = S.ring("ropeS", 1, [128, SBW], F32)
        ssr = S.ring("ss", 2, [128, 1], F32)
        sdr = S.ring("sd", 2, [128, 1], F32)
        rsr = S.ring("rs", 2, [128, 1], F32)
        sqr = S.ring("sq", 2, [128, 512], BF16)
        sdtr = S.ring("sdt", 2, [128, 512], F32)
        qnr = S.ring("qn", 2, [128, 512], BF16)
        t1r = S.ring("t1", 2, [128, 512], F32)
        t2r = S.ring("t2", 2, [128, 512], F32)
        qfr = S.ring("qf", 2, [128, 512], BF16)
        ogr = S.ring("og", 2, [128, 512], BF16)
        abr = S.ring("ab", 2, [128, 4, 512], BF16)
        pt, ptb = S.ps("pt", [128, 2048], BF16)
        pjr = S.ring("pj", 3, [128, 512], F32, psum=True)
        pm, pmb = S.ps("pm", [128, 512], F32)
        pr, prb = S.ps("pr", [128, 512], F32)
        pdr = S.ring("pd", 1, [128, 512], F32, psum=True)
        epsc, epscb = S.sb("epsc", [128, 1], F32)
        c["epsc"], cb["epsc"] = epsc, epscb
        ctx.op(DVE, lambda e: e.memset(epsc[:], EPS), writes=[epscb])

        class SB:
            pass
        sbs = []

        def mk(src, W, meta, si, t0):
            o = SB()
            o.src, o.W, o.meta, o.si, o.t0 = src, W, meta, si, t0
            o.TW = min(128, W)
            o.ntile = W // o.TW
            o.BW = min(512, W)
            o.nblk = W // o.BW
            o.tpb = o.BW // o.TW
            o.hnT, o.hnTb = hn_slots[len(sbs) % 2]
            o.wbs = [wb for wb in range(16) if not (meta and (2 <= wb < 6 or wb >= 8))]
            o.xn = {}
            sbs.append(o)
        mk(G.din["meta"], NMETA, True, -1, 0)
        for si, n in enumerate(G.seq_ns):
            for t0 in range(0, n, SBW):
                mk(G.din[f"x{si}"][t0:t0 + SBW, :], SBW, False, si, t0)

        wtasks = [(o, wb) for o in sbs for wb in o.wbs]
        wslots = {}
        wstate = {"issued": 0}

        def wget(k):
            while wstate["issued"] < min(len(wtasks), k + 3):
                kk = wstate["issued"]
                wt, wbuf = wring.next()
                wb = wtasks[kk][1]
                ctx.dma(SP, lambda e: e.dma_start(out=wt[:], in_=w_in_r[:, :, wb * 512:(wb + 1) * 512]),
                        sb=wbuf, reads=[G.wbb[("w_in", wb)]], writes=[wbuf])
                wslots[kk] = (wt, wbuf)
                wstate["issued"] += 1
            return wslots.pop(k)

        def a1_prep(o, ti):
            TW = o.TW
            xt, xb = xring.next()
            ctx.dma(SP, lambda e: e.dma_start(out=xt[0:TW, :], in_=o.src[ti * TW:(ti + 1) * TW, :]),
                    sb=xb, writes=[xb])
            ss, ssb = ssr.next()
            xn, xnb = xnring.next()
            ctx.op(ACT, lambda e: e.activation(out=xn[0:TW, :], in_=xt[0:TW, :], func=AF.Square,
                                               accum_out=ss[0:TW, 0:1]),
                   reads=[xb], writes=[xnb, ssb])
            sd, sdb = sdr.next()
            ctx.op(ACT, lambda e: e.activation(out=sd[0:TW, :], in_=ss[0:TW, :], func=AF.Sqrt,
                                               bias=epsc[0:TW, 0:1], scale=1.0 / D),
                   reads=[ssb, epscb], writes=[sdb])
            rs, rsb = rsr.next()
            ctx.op(DVE, lambda e: e.reciprocal(out=rs[0:TW, :], in_=sd[0:TW, :]), reads=[sdb], writes=[rsb])
            ctx.op(DVE, lambda e: e.scalar_tensor_tensor(out=xn[0:TW, :], in0=xt[0:TW, :], scalar=rs[0:TW, 0:1],
                                                         in1=g1b[0:TW, :], op0=ALU.mult, op1=ALU.mult),
                   reads=[xb, rsb, g1bb], writes=[xnb])
            o.xn[ti] = (xn, xnb)

        def a1_trans(o, ti):
            TW = o.TW
            xn, xnb = o.xn.pop(ti)
            ctx.begin(PE, reads=[xnb, cb["ident_b"]], writes=[ptb])
            for cc in range(16):
                ins = nc.tensor.transpose(out=pt[:, cc * 128:cc * 128 + TW], in_=xn[0:TW, cc * 128:(cc + 1) * 128],
                                          identity=c["ident_b"][0:TW, 0:TW])
            ctx.end(PE, ins, reads=[xnb, cb["ident_b"]], writes=[ptb])
            ptv = pt[:, :].rearrange("p (c t) -> p c t", c=16)
            ctx.op(ACT, lambda e: e.copy(out=o.hnT[:, :, ti * TW:(ti + 1) * TW], in_=ptv[:, :, 0:TW]),
                   reads=[ptb], writes=[o.hnTb[ti]])

        def a1_steps(o):
            steps = []
            for k in range(o.ntile + 1):
                def st(k=k):
                    if k < o.ntile:
                        a1_prep(o, k)
                    if k >= 1:
                        a1_trans(o, k - 1)
                steps.append(st)
            return steps

        def a2(o, wk0, hooks):
            meta, si, t0, TW, BW, W = o.meta, o.si, o.t0, o.TW, o.BW, o.W
            ntile, nblk, tpb = o.ntile, o.nblk, o.tpb
            hnT, hnTb = o.hnT, o.hnTb
            if not meta:
                n = G.seq_ns[si]
                ropeC, ropeCb = ropeCr.next()
                ropeS, ropeSb = ropeSr.next()
                ctx.dma(SP, lambda e: e.dma_start(out=ropeC[:, 0:W], in_=G.din[f"ropec_{n}"][:, t0:t0 + W]),
                        sb=ropeCb, writes=[ropeCb])
                ctx.dma(SP, lambda e: e.dma_start(out=ropeS[:, 0:W], in_=G.din[f"ropes_{n}"][:, t0:t0 + W]),
                        sb=ropeSb, writes=[ropeSb])

            q1, q2 = [], []

            def tick():
                run = list(q2)
                q2.clear()
                for f in run:
                    f()
                run = list(q1)
                q1.clear()
                for f in run:
                    f()

            def qk_pipeline(ps, psb, gain, gainb, dst_fn, blk):
                sq, sqb = sqr.next()
                ctx.op(ACT, lambda e: e.activation(out=sq[:, 0:BW], in_=ps[:, 0:BW], func=AF.Square),
                       reads=[psb], writes=[sqb])

                def stage1():
                    ctx.begin(PE, reads=[sqb, cb["onesm_b"]], writes=[pmb])
                    ins = nc.tensor.matmul(pm[:, 0:BW], c["onesm_b"][:, :], sq[:, 0:BW], start=True, stop=True)
                    ctx.end(PE, ins, reads=[sqb, cb["onesm_b"]], writes=[pmb])
                    sdt, sdtb = sdtr.next()
                    ctx.op(ACT, lambda e: e.activation(out=sdt[:, 0:BW], in_=pm[:, 0:BW], func=AF.Sqrt,
                                                       bias=epsc[:, 0:1], scale=1.0),
                           reads=[pmb, epscb], writes=[sdtb])
                    rst, rstb = sdt, sdtb
                    ctx.op(DVE, lambda e: e.reciprocal(out=rst[:, 0:BW], in_=sdt[:, 0:BW]), reads=[sdtb],
                           writes=[rstb])
                    qn, qnb = qnr.next()
                    ctx.op(DVE, lambda e: e.scalar_tensor_tensor(out=qn[:, 0:BW], in0=ps[:, 0:BW],
                                                                 scalar=gain[:, 0:1], in1=rst[:, 0:BW],
                                                                 op0=ALU.mult, op1=ALU.mult),
                           reads=[psb, gainb, rstb], writes=[qnb])
                    if meta:
                        dst_fn(qn, qnb)
                        return

                    def stage2():
                        ctx.begin(PE, reads=[qnb, cb["rotT_b"]], writes=[prb])
                        ins = nc.tensor.matmul(pr[:, 0:BW], c["rotT_b"][:, :], qn[:, 0:BW], start=True, stop=True)
                        ctx.end(PE, ins, reads=[qnb, cb["rotT_b"]], writes=[prb])
                        t1, t1b = t1r.next()
                        ctx.op(POOL, lambda e: e.tensor_tensor(out=t1[:, 0:BW], in0=qn[:, 0:BW],
                                                               in1=ropeC[:, blk * BW:(blk + 1) * BW], op=ALU.mult),
                               reads=[qnb, ropeCb], writes=[t1b])
                        t2, t2b = t2r.next()
                        ctx.op(DVE, lambda e: e.tensor_tensor(out=t2[:, 0:BW], in0=pr[:, 0:BW],
                                                              in1=ropeS[:, blk * BW:(blk + 1) * BW], op=ALU.mult),
                               reads=[prb, ropeSb], writes=[t2b])
                        qf, qfb = qfr.next()
                        ctx.op(POOL, lambda e: e.tensor_tensor(out=qf[:, 0:BW], in0=t1[:, 0:BW], in1=t2[:, 0:BW],
                                                               op=ALU.add),
                               reads=[t1b, t2b], writes=[qfb])
                        dst_fn(qf, qfb)
                    q2.append(stage2)
                q1.append(stage1)

            for wi, wb in enumerate(o.wbs):
                kind = "u" if wb < 2 else "q" if wb < 6 else "k" if wb == 6 else "v" if wb == 7 else \
                    "ga" if wb < 12 else "gf"
                wt, wbuf = wget(wk0 + wi)
                if kind == "v":
                    for ti in range(ntile):
                        ps, psb = pjr.next()
                        rd = [hnTb[ti], wbuf]
                        ctx.begin(PE, reads=rd, writes=[psb])
                        for cc in range(16):
                            ins = nc.tensor.matmul(ps[0:TW, :], hnT[:, cc, ti * TW:(ti + 1) * TW], wt[:, cc, :],
                                                   start=(cc == 0), stop=(cc == 15))
                        ctx.end(PE, ins, reads=rd, writes=[psb])
                        tick()
                        st, stb = ogr.next()
                        ctx.op(ACT, lambda e: e.copy(out=st[0:TW, :], in_=ps[0:TW, :]), reads=[psb], writes=[stb])
                        if meta:
                            dst, dname = G.scr["vm"][:, :], "vm"
                        else:
                            dst, dname = G.scr[f"v{si}"][t0 + ti * TW:t0 + (ti + 1) * TW, :], f"v{si}"
                        ctx.dma(SP, lambda e: e.dma_start(out=dst, in_=st[0:TW, :]), sb=stb, reads=[stb],
                                writes=[G.dbuf[dname]])
                    hooks(wi)
                    continue
                for blk in range(nblk):
                    for ch in range(4):
                        ps, psb = pjr.next()
                        rd = [hnTb[blk * tpb + j] for j in range(tpb)] + [wbuf]
                        ctx.begin(PE, reads=rd, writes=[psb])
                        for cc in range(16):
                            ins = nc.tensor.matmul(ps[:, 0:BW], wt[:, cc, ch * 128:(ch + 1) * 128],
                                                   hnT[:, cc, blk * BW:(blk + 1) * BW],
                                                   start=(cc == 0), stop=(cc == 15))
                        ctx.end(PE, ins, reads=rd, writes=[psb])
                        tick()
                        c0 = t0 + blk * BW
                        if kind == "u":
                            uc = wb * 4 + ch
                            ctx.op(ACT, lambda e: e.copy(out=uT[:, uc, blk * BW:(blk + 1) * BW], in_=ps[:, 0:BW]),
                                   reads=[psb], writes=[uTb[blk]])
                        elif kind in ("q", "k"):
                            hh = (wb - 2) * 4 + ch if kind == "q" else ch
                            if kind == "q":
                                dst, dname = G.scr[f"qT{si}"][hh, :, c0:c0 + BW], f"qT{si}"
                            elif meta:
                                dst, dname = G.scr["kTm"][hh, :, :], "kTm"
                            else:
                                dst, dname = G.scr[f"kT{si}"][hh, :, c0:c0 + BW], f"kT{si}"

                            def dst_fn(tl, tlb, dst=dst, dname=dname):
                                ctx.dma(SP, lambda e: e.dma_start(out=dst, in_=tl[:, 0:BW]), sb=tlb, reads=[tlb],
                                        writes=[G.dbuf[dname]])
                            gname = "qg" if kind == "q" else "kg"
                            qk_pipeline(ps, psb, c[gname], cb[gname], dst_fn, blk)
                        else:
                            st, stb = ogr.next()
                            ctx.op(ACT, lambda e: e.activation(out=st[:, 0:BW], in_=ps[:, 0:BW], func=AF.Sigmoid),
                                   reads=[psb], writes=[stb])
                            base = (wb - 8) * 512 if kind == "ga" else (wb - 12) * 512
                            dname = f"sga{si}" if kind == "ga" else f"sgf{si}"
                            dst = G.scr[dname][base + ch * 128:base + (ch + 1) * 128, c0:c0 + BW]
                            ctx.dma(SP, lambda e: e.dma_start(out=dst, in_=st[:, 0:BW]), sb=stb, reads=[stb],
                                    writes=[G.dbuf[dname]])
                if kind == "u" and wb == 1:
                    for ti in range(ntile):
                        abt, abtb = abr.next()
                        for g in range(4):
                            pd, pdb = pdr.next()
                            rd = [uTb[ti // tpb], cb["cc_b"]]
                            ctx.begin(PE, reads=rd, writes=[pdb])
                            for kc in range(2):
                                ins = nc.tensor.matmul(pd[0:TW, :], uT[:, 2 * g + kc, ti * TW:(ti + 1) * TW],
                                                       c["cc_b"][:, kc, :], start=(kc == 0), stop=(kc == 1))
                            ctx.end(PE, ins, reads=rd, writes=[pdb])
                            ctx.op(DVE, lambda e: e.tensor_copy(out=abt[0:TW, g, :], in_=pd[0:TW, :]),
                                   reads=[pdb], writes=[abtb])
                        if meta:
                            dst, dname = G.scr["ABm"][:, :], "ABm"
                        else:
                            dst, dname = G.scr[f"AB{si}"][t0 + ti * TW:t0 + (ti + 1) * TW, :], f"AB{si}"
                        dstv = dst.rearrange("t (g f) -> t g f", g=4)
                        ctx.dma(SP, lambda e: e.dma_start(out=dstv, in_=abt[0:TW, :, :]), sb=abtb, reads=[abtb],
                                writes=[G.dbuf[dname]])
                hooks(wi)
            tick()
            tick()

        for st in a1_steps(sbs[0]):
            st()
        wk = 0
        for i, o in enumerate(sbs):
            nxt = a1_steps(sbs[i + 1]) if i + 1 < len(sbs) else []
            pos = {"k": 0}

            def hooks(wi, nxt=nxt, pos=pos):
                if wi >= 1 and pos["k"] < len(nxt):
                    nxt[pos["k"]]()
                    pos["k"] += 1
            a2(o, wk, hooks)
            wk += len(o.wbs)
            while pos["k"] < len(nxt):
                nxt[pos["k"]]()
                pos["k"] += 1


def phase_F(G, si):
    nc, ctx = G.nc, G.ctx
    PE, ACT, DVE, POOL, SP = ctx.PE, ctx.ACT, ctx.DVE, ctx.POOL, ctx.SP
    n = G.seq_ns[si]
    LB, GB = lb_for(n), gb_for(n)
    nt = 1 + n // 128
    ABd = G.scr[f"AB{si}"].rearrange("(t p) f -> p t f", p=128)
    dft = G.din[f"dft_{n}"]
    with Scope(ctx) as S:
        ABs, ABsb = S.sb("ABs", [128, nt, GB * 512], BF16)
        csr = S.ring("cs", 2, [128, nt, 2, LB], BF16)
        ztr = S.ring("zt", 3, [128, LB], BF16)
        pzr = S.ring("pz", 4, [128, 512], F32, psum=True)
        k_ev = 0
        for gs in range(0, 4, GB):
            for gi in range(GB):
                g = gs + gi
                for ta in range(0, nt - 1, 8):
                    tb_ = min(nt - 1, ta + 8)
                    ctx.dma(SP, lambda e: e.dma_start(out=ABs[:, 1 + ta:1 + tb_, gi * 512:(gi + 1) * 512],
                                                      in_=ABd[:, ta:tb_, g * 512:(g + 1) * 512]),
                            sb=ABsb, reads=[G.dbuf[f"AB{si}"]], writes=[ABsb])
                ctx.dma(SP, lambda e: e.dma_start(out=ABs[0:NMETA, 0, gi * 512:(gi + 1) * 512],
                                                  in_=G.scr["ABm"][:, g * 512:(g + 1) * 512]),
                        sb=ABsb, reads=[G.dbuf["ABm"]], writes=[ABsb])
            for j in range(n // LB):
                cs, csb = csr.next()
                ctx.dma(SP, lambda e: e.dma_start(out=cs[:], in_=dft[j]), sb=csb, writes=[csb])
                for gi in range(GB):
                    g = gs + gi
                    for cc in range(2):
                        ps, psb = pzr.next()
                        rd = [ABsb, csb]
                        ctx.begin(PE, reads=rd, writes=[psb])
                        k = 0
                        for t in range(nt):
                            rows = NMETA if t == 0 else 128
                            for ab in range(2):
                                col = gi * 512 + ab * 256 + cc * 128
                                ins = nc.tensor.matmul(ps[:, 0:LB], ABs[0:rows, t, col:col + 128],
                                                       cs[0:rows, t, ab, :], start=(k == 0),
                                                       stop=(k == 2 * nt - 1))
                                k += 1
                        ctx.end(PE, ins, reads=rd, writes=[psb])
                        zt, ztb = ztr.next()
                        if k_ev % 2 == 0:
                            ctx.op(ACT, lambda e: e.copy(out=zt[:, 0:LB], in_=ps[:, 0:LB]), reads=[psb], writes=[ztb])
                        else:
                            ctx.op(DVE, lambda e: e.tensor_copy(out=zt[:, 0:LB], in_=ps[:, 0:LB]), reads=[psb],
                                   writes=[ztb])
                        k_ev += 1
                        r0 = g * 256 + cc * 128
                        ctx.dma(SP, lambda e: e.dma_start(out=G.scr[f"ZT{si}"][r0:r0 + 128, j * LB:(j + 1) * LB],
                                                          in_=zt[:, 0:LB]),
                                sb=ztb, reads=[ztb], writes=[G.dbuf[f"ZT{si}"]])


def phase_At(G, si):
    nc, ctx = G.nc, G.ctx
    PE, ACT, DVE, POOL, SP = ctx.PE, ctx.ACT, ctx.DVE, ctx.POOL, ctx.SP
    c, cb = G.c, G.cb
    n = G.seq_ns[si]
    L = n + NMETA
    nt = 1 + n // 128
    npair = (nt - 1) // 2
    QB = min(512, n)
    vd = G.scr[f"v{si}"].rearrange("(t p) f -> p t f", p=128)
    scale = float(HD) ** -0.5
    with Scope(ctx) as S:
        kr = S.ring("kT", 2, [128, L], BF16)
        vr = S.ring("V", 2, [128, nt, 128], BF16)
        qr = S.ring("q", 3, [128, QB], BF16)
        pmr = S.ring("pm", 2, [128, QB], BF16)
        ppr = S.ring("pp", 4, [128, 2, QB], BF16)
        accAr = S.ring("accA", 2, [128, 2, QB], F32)
        accBr = S.ring("accB", 2, [128, 2, QB], F32)
        acsr = S.ring("acs", 2, [128, 2, QB], BF16)
        rcr = S.ring("rc", 2, [128, QB], F32)
        atr = S.ring("at", 2, [128, QB], BF16)
        spr = S.ring("sp", 2, [128, 1024], F32, psum=True)
        otr = S.ring("ot", 2, [128, 512], F32, psum=True)
        rsfr = S.ring("rsf", 2, [128, 512], F32, psum=True)
        pending_fin = []
        dsel = [0]
        pe_pairs = [pr for pr in range(npair) if pr % 5 in (1, 3)]

        def run_fin():
            while pending_fin:
                pending_fin.pop(0)()

        for kh in range(NKV):
            kt, ktb = kr.next()
            vt, vtb = vr.next()
            ctx.dma(SP, lambda e: e.dma_start(out=kt[:, NMETA:L], in_=G.scr[f"kT{si}"][kh]), sb=ktb,
                    reads=[G.dbuf[f"kT{si}"]], writes=[ktb])
            ctx.dma(SP, lambda e: e.dma_start(out=kt[:, 0:NMETA], in_=G.scr["kTm"][kh]), sb=ktb,
                    reads=[G.dbuf["kTm"]], writes=[ktb])
            for ta in range(0, nt - 1, 8):
                tb_ = min(nt - 1, ta + 8)
                ctx.dma(SP, lambda e: e.dma_start(out=vt[:, 1 + ta:1 + tb_, :],
                                                  in_=vd[:, ta:tb_, kh * 128:(kh + 1) * 128]), sb=vtb,
                        reads=[G.dbuf[f"v{si}"]], writes=[vtb])
            ctx.dma(SP, lambda e: e.dma_start(out=vt[0:NMETA, 0, :], in_=G.scr["vm"][:, kh * 128:(kh + 1) * 128]),
                    sb=vtb, reads=[G.dbuf["vm"]], writes=[vtb])
            for hq in range(4):
                h = kh * 4 + hq
                for qb in range(n // QB):
                    qt, qtb = qr.next()
                    ctx.dma(SP, lambda e: e.dma_start(out=qt[:], in_=G.scr[f"qT{si}"][h, :, qb * QB:(qb + 1) * QB]),
                            sb=qtb, reads=[G.dbuf[f"qT{si}"]], writes=[qtb])
                    ot, otb = otr.next()
                    rsf, rsfb = rsfr.next()
                    accA, accAb = accAr.next()
                    accB, accBb = accBr.next()
                    ctx.op(POOL, lambda e: e.memset(accA[:], 0.0), writes=[accAb])
                    ctx.op(POOL, lambda e: e.memset(accB[:], 0.0), writes=[accBb])

                    def pv_meta(pm, pmb, ot=ot, otb=otb, accA=accA, accAb=accAb, vt=vt, vtb=vtb):
                        rd = [vtb, pmb]
                        ctx.begin(PE, reads=rd, writes=[otb])
                        ins = nc.tensor.matmul(ot[:, 0:QB], vt[0:NMETA, 0, :], pm[0:NMETA, :], start=True, stop=False)
                        ctx.end(PE, ins, reads=rd, writes=[otb])
                        ctx.op(DVE, lambda e: e.tensor_tensor(out=accA[0:NMETA, 0, :], in0=accA[0:NMETA, 0, :],
                                                              in1=pm[0:NMETA, :], op=ALU.add),
                               reads=[pmb, accAb], writes=[accAb])

                    def pv_pair(pr, pp, ppb, ot=ot, otb=otb, accA=accA, accAb=accAb, accB=accB, accBb=accBb,
                                vt=vt, vtb=vtb, rsf=rsf, rsfb=rsfb):
                        on_pe = pr in pe_pairs
                        rd = [vtb, ppb] + ([cb["ones_b"]] if on_pe else [])
                        wr = [otb] + ([rsfb] if on_pe else [])
                        ctx.begin(PE, reads=rd, writes=wr)
                        nc.tensor.matmul(ot[:, 0:QB], vt[:, 1 + 2 * pr, :], pp[:, 0, :], start=False, stop=False)
                        ins = nc.tensor.matmul(ot[:, 0:QB], vt[:, 2 + 2 * pr, :], pp[:, 1, :], start=False,
                                               stop=(pr == npair - 1))
                        if on_pe:
                            nc.tensor.matmul(rsf[:, 0:QB], c["ones_b"][:, :], pp[:, 0, :], start=(pr == pe_pairs[0]),
                                             stop=False)
                            ins = nc.tensor.matmul(rsf[:, 0:QB], c["ones_b"][:, :], pp[:, 1, :], start=False,
                                                   stop=False)
                        ctx.end(PE, ins, reads=rd, writes=wr)
                        if on_pe:
                            return
                        dsel[0] += 1
                        ac, acb = (accA, accAb) if dsel[0] % 2 == 1 else (accB, accBb)
                        ctx.op(DVE, lambda e: e.tensor_tensor(out=ac[:, :, :], in0=ac[:, :, :], in1=pp[:, :, :],
                                                              op=ALU.add),
                               reads=[ppb, acb], writes=[acb])

                    def fin(ot=ot, otb=otb, accA=accA, accAb=accAb, accB=accB, accBb=accBb, h=h, qb=qb,
                            rsf=rsf, rsfb=rsfb):
                        acs, acsb = acsr.next()
                        ctx.op(POOL, lambda e: e.tensor_tensor(out=acs[:, :, :], in0=accA[:, :, :], in1=accB[:, :, :],
                                                               op=ALU.add),
                               reads=[accAb, accBb], writes=[acsb])
                        rd = [acsb, cb["ones_b"]]
                        ctx.begin(PE, reads=rd, writes=[rsfb])
                        nc.tensor.matmul(rsf[:, 0:QB], c["ones_b"][:, :], acs[:, 0, :], start=(len(pe_pairs) == 0),
                                         stop=False)
                        ins = nc.tensor.matmul(rsf[:, 0:QB], c["ones_b"][:, :], acs[:, 1, :], start=False, stop=True)
                        ctx.end(PE, ins, reads=rd, writes=[rsfb])
                        rc, rcb = rcr.next()
                        ctx.op(DVE, lambda e: e.reciprocal(out=rc[:, :], in_=rsf[:, 0:QB]), reads=[rsfb], writes=[rcb])
                        at, atb = atr.next()
                        ctx.op(DVE, lambda e: e.tensor_tensor(out=at[:, :], in0=ot[:, 0:QB], in1=rc[:, :],
                                                              op=ALU.mult),
                               reads=[otb, rcb], writes=[atb])
                        ctx.dma(SP, lambda e: e.dma_start(out=G.scr[f"attnT{si}"][h * 128:(h + 1) * 128,
                                                                                 qb * QB:(qb + 1) * QB],
                                                          in_=at[:, :]),
                                sb=atb, reads=[atb], writes=[G.dbuf[f"attnT{si}"]])

                    sp, spb = spr.next()
                    rd = [ktb, qtb]
                    ctx.begin(PE, reads=rd, writes=[spb])
                    ins = nc.tensor.matmul(sp[0:NMETA, 0:QB], kt[:, 0:NMETA], qt[:, :], start=True, stop=True)
                    ctx.end(PE, ins, reads=rd, writes=[spb])
                    pm, pmb = pmr.next()
                    ctx.op(ACT, lambda e: e.activation(out=pm[0:NMETA, :], in_=sp[0:NMETA, 0:QB], func=AF.Exp,
                                                       scale=scale), reads=[spb], writes=[pmb])
                    pend = (pv_meta, (pm, pmb))
                    for pr in range(npair):
                        c0 = NMETA + pr * 256
                        sp, spb = spr.next()
                        rd = [ktb, qtb]
                        ctx.begin(PE, reads=rd, writes=[spb])
                        nc.tensor.matmul(sp[:, 0:QB], kt[:, c0:c0 + 128], qt[:, :], start=True, stop=True)
                        ins = nc.tensor.matmul(sp[:, 512:512 + QB], kt[:, c0 + 128:c0 + 256], qt[:, :], start=True,
                                               stop=True)
                        ctx.end(PE, ins, reads=rd, writes=[spb])
                        pp, ppb = ppr.next()
                        spv = sp[:, :].rearrange("p (a q) -> p a q", a=2)[:, :, 0:QB]
                        ctx.op(ACT, lambda e: e.activation(out=pp[:, :, :], in_=spv, func=AF.Exp, scale=scale),
                               reads=[spb], writes=[ppb])
                        pend[0](*pend[1])
                        pend = (pv_pair, (pr, pp, ppb))
                        if pr == min(2, npair - 1):
                            run_fin()
                    pend[0](*pend[1])
                    pending_fin.append(fin)
        run_fin()


def phase_G1(G):
    nc, ctx = G.nc, G.ctx
    PE, ACT, DVE, POOL, SP = ctx.PE, ctx.ACT, ctx.DVE, ctx.POOL, ctx.SP
    BW = 256
    w_ao_r = G.wb["w_ao"].rearrange("(c p) n -> p c n", p=128)
    w_fo_r = G.wb["w_fo"].rearrange("(c p) n -> p c n", p=128)
    with Scope(ctx) as S:
        wao, waob = S.sb("wao", [128, 16, D], BF16)
        wfo, wfob = S.sb("wfo", [128, 8, D], BF16)
        for q4 in range(4):
            ctx.dma(SP, lambda e: e.dma_start(out=wao[:, :, q4 * 512:(q4 + 1) * 512],
                                              in_=w_ao_r[:, :, q4 * 512:(q4 + 1) * 512]), sb=waob,
                    reads=[G.wbb[("w_ao", q4)]], writes=[waob])
            ctx.dma(SP, lambda e: e.dma_start(out=wfo[:, :, q4 * 512:(q4 + 1) * 512],
                                              in_=w_fo_r[:, :, q4 * 512:(q4 + 1) * 512]), sb=wfob,
                    reads=[G.wbb[("w_fo", q4)]], writes=[wfob])
        atr = S.ring("at", 2, [128, 16, BW], BF16)
        ztr = S.ring("zt", 2, [128, 8, BW], BF16)
        gar = S.ring("ga", 2, [128, 16, BW], BF16)
        gfr = S.ring("gf", 2, [128, 16, BW], BF16)
        mtr = S.ring("mt", 2, [128, 16, BW], BF16)
        t1r = S.ring("t1", 2, [128, BW], F32)
        t2r = S.ring("t2", 2, [128, BW], F32)
        par = S.ring("pa", 3, [128, 512], F32, psum=True)
        pfr = S.ring("pf", 3, [128, 512], F32, psum=True)
        for si, n in enumerate(G.seq_ns):
            for b0 in range(0, n, BW):
                at, atb = atr.next()
                zt, ztb = ztr.next()
                ga, gab = gar.next()
                gf, gfb_ = gfr.next()
                for (tl, tlb, nm) in ((at, atb, f"attnT{si}"), (zt, ztb, f"ZT{si}"), (ga, gab, f"sga{si}"),
                                      (gf, gfb_, f"sgf{si}")):
                    srcv = G.scr[nm].rearrange("(c p) t -> p c t", p=128)[:, :, b0:b0 + BW]
                    ctx.dma(SP, lambda e: e.dma_start(out=tl[:], in_=srcv), sb=tlb, reads=[G.dbuf[nm]], writes=[tlb])
                mt, mtb = mtr.next()
                for fc in range(16):
                    pa, pab = par.next()
                    rd = [waob, atb]
                    ctx.begin(PE, reads=rd, writes=[pab])
                    for h in range(16):
                        ins = nc.tensor.matmul(pa[:, 0:BW], wao[:, h, fc * 128:(fc + 1) * 128], at[:, h, :],
                                               start=(h == 0), stop=(h == 15))
                    ctx.end(PE, ins, reads=rd, writes=[pab])
                    pf, pfb = pfr.next()
                    rd = [wfob, ztb]
                    ctx.begin(PE, reads=rd, writes=[pfb])
                    for zc in range(8):
                        ins = nc.tensor.matmul(pf[:, 0:BW], wfo[:, zc, fc * 128:(fc + 1) * 128], zt[:, zc, :],
                                               start=(zc == 0), stop=(zc == 7))
                    ctx.end(PE, ins, reads=rd, writes=[pfb])
                    t1, t1b = t1r.next()
                    ctx.op(DVE, lambda e: e.tensor_tensor(out=t1[:, :], in0=pa[:, 0:BW], in1=ga[:, fc, :], op=ALU.mult),
                           reads=[pab, gab], writes=[t1b])
                    t2, t2b = t2r.next()
                    ctx.op(DVE, lambda e: e.tensor_tensor(out=t2[:, :], in0=pf[:, 0:BW], in1=gf[:, fc, :], op=ALU.mult),
                           reads=[pfb, gfb_], writes=[t2b])
                    ctx.op(POOL, lambda e: e.tensor_tensor(out=mt[:, fc, :], in0=t1[:, :], in1=t2[:, :], op=ALU.add),
                           reads=[t1b, t2b], writes=[mtb])
                dstv = G.scr[f"mT{si}"].rearrange("(c p) t -> p c t", p=128)[:, :, b0:b0 + BW]
                ctx.dma(SP, lambda e: e.dma_start(out=dstv, in_=mt[:]), sb=mtb, reads=[mtb],
                        writes=[G.dbuf[f"mT{si}"]])


BIG = 1.0e4


def phase_G2(G):
    nc, ctx = G.nc, G.ctx
    PE, ACT, DVE, POOL, SP = ctx.PE, ctx.ACT, ctx.DVE, ctx.POOL, ctx.SP
    c, cb = G.c, G.cb
    w_out_r = G.wb["w_out"].rearrange("(c p) n -> p c n", p=128)
    w_r_r = G.din["w_r"].rearrange("(c p) n -> p c n", p=128)
    with Scope(ctx) as S:
        wout, woutb = S.sb("wout", [128, 16, D], BF16)
        for q4 in range(4):
            ctx.dma(SP, lambda e: e.dma_start(out=wout[:, :, q4 * 512:(q4 + 1) * 512],
                                              in_=w_out_r[:, :, q4 * 512:(q4 + 1) * 512]), sb=woutb,
                    reads=[G.wbb[("w_out", q4)]], writes=[woutb])
        wr, wrb = S.sb("wr", [128, 16, 36], F32)
        ctx.dma(SP, lambda e: e.dma_start(out=wr[:], in_=w_r_r), sb=wrb, writes=[wrb])
        g2b, g2bb = S.sb("g2b", [128, D], F32)
        ctx.dma(SP, lambda e: e.dma_start(out=g2b[:], in_=G.din["g2b"]), sb=g2bb, writes=[g2bb])
        epsc, epscb = S.sb("epsc", [128, 1], F32)
        ctx.op(DVE, lambda e: e.memset(epsc[:], EPS), writes=[epscb])
        selacc, selaccb = S.sb("selacc", [128, NE], F32)
        ctx.op(DVE, lambda e: e.memset(selacc[:], 0.0), writes=[selaccb])
        mtr = S.ring("mt", 2, [128, 16, 512], BF16)
        xr = S.ring("x", 2, [128, D], F32)
        h1r = S.ring("h1", 2, [128, D], F32)
        junk, junkb = S.sb("junk", [128, D], BF16)
        hnr = S.ring("hn", 3, [128, D], F32)
        hbr = S.ring("hb", 4, [128, D], BF16)
        hT, hTb = S.sb("hT", [128, 16, 128], F32)
        por = S.ring("po", 4, [128, 512], F32, psum=True)
        ptr_ = S.ring("ptr", 2, [128, 512], F32, psum=True)
        plog, plogb = S.ps("plog", [128, 512], F32)
        prank, prankb = S.ps("prank", [128, 512], F32)

        def small(name, w):
            return S.sb(name, [128, w], F32)
        ss, ssb = small("ss", 1)
        sd, sdb = small("sd", 1)
        rs, rsb = small("rs", 1)
        lg, lgb = small("lg", 36)
        gmax, gmaxb = small("gmax", 1)
        ngmax, ngmaxb = small("ngmax", 1)
        gm, gmb = small("gm", 4)
        eg, egb = small("eg", 4)
        se, seb = small("se", 1)
        pg, pgb = small("pg", 1)
        pen, penb = small("pen", 4)
        lm, lmb = small("lm", NE)
        m1, m1b = small("m1", 1)
        mk1, mk1b = small("mk1", NE)
        lm2, lm2b = small("lm2", NE)
        m2, m2b = small("m2", 1)
        mk2, mk2b = small("mk2", NE)
        dd, ddb = small("dd", 1)
        w1, w1b = small("w1", 1)
        w2, w2b = small("w2", 1)
        sel, selb = small("sel", NE)
        rk, rkb = small("rk", NE)
        ov, ovb = small("ov", NE)
        sm, smb = small("sm", NE)
        tm, tmb = small("tm", NE)
        sf1, sf1b = small("sf1", 1)
        sf2, sf2b = small("sf2", 1)

        tiles = []
        tile_g = 0
        for si, n in enumerate(G.seq_ns):
            BW = min(512, n)
            for b0 in range(0, n, BW):
                for tt in range(BW // 128):
                    tiles.append((si, n, BW, b0, tt, b0 + tt * 128, tile_g))
                    tile_g += 1
        st = {}
        cur = {}

        hst = {}

        def dv(fn, reads, writes):
            ctx.op(DVE, fn, reads=reads, writes=writes)

        def sa(ti):
            si, n, BW, b0, tt, r0, tg = tiles[ti]
            if tt == 0:
                mTd = G.scr[f"mT{si}"].rearrange("(c p) t -> p c t", p=128)
                mt, mtb = mtr.next()
                ctx.dma(SP, lambda e: e.dma_start(out=mt[:, :, 0:BW], in_=mTd[:, :, b0:b0 + BW]), sb=mtb,
                        reads=[G.dbuf[f"mT{si}"]], writes=[mtb])
                cur["mt"] = (mt, mtb)
            mt, mtb = cur["mt"]
            xt, xb = xr.next()
            ctx.dma(SP, lambda e: e.dma_start(out=xt[:], in_=G.din[f"x{si}"][r0:r0 + 128, :]), sb=xb,
                    writes=[xb])
            h1, h1b = h1r.next()
            for nb in range(4):
                po, pob = por.next()
                rd = [mtb, woutb]
                ctx.begin(PE, reads=rd, writes=[pob])
                for fc in range(16):
                    ins = nc.tensor.matmul(po[:, :], mt[:, fc, tt * 128:(tt + 1) * 128],
                                           wout[:, fc, nb * 512:(nb + 1) * 512], start=(fc == 0),
                                           stop=(fc == 15))
                ctx.end(PE, ins, reads=rd, writes=[pob])
                ctx.op(DVE, lambda e: e.tensor_tensor(out=h1[:, nb * 512:(nb + 1) * 512], in0=po[:, :],
                                                      in1=xt[:, nb * 512:(nb + 1) * 512], op=ALU.add),
                       reads=[pob, xb], writes=[h1b])
            ctx.dma(SP, lambda e: e.dma_start(out=G.scr[f"h1_{si}"][r0:r0 + 128, :], in_=h1[:]), sb=h1b,
                    reads=[h1b], writes=[G.dbuf[f"h1_{si}"]])
            hst[ti] = (h1, h1b)

        def sb_(ti):
            si, n, BW, b0, tt, r0, tg = tiles[ti]
            h1, h1b = hst.pop(ti)
            ctx.op(ACT, lambda e: e.activation(out=junk[:], in_=h1[:], func=AF.Square, accum_out=ss[:, 0:1]),
                   reads=[h1b], writes=[junkb, ssb])
            ctx.op(ACT, lambda e: e.activation(out=sd[:], in_=ss[:], func=AF.Sqrt, bias=epsc[:, 0:1],
                                               scale=1.0 / D), reads=[ssb, epscb], writes=[sdb])
            ctx.op(DVE, lambda e: e.reciprocal(out=rs[:], in_=sd[:]), reads=[sdb], writes=[rsb])
            hn, hnb = hnr.next()
            ctx.op(DVE, lambda e: e.scalar_tensor_tensor(out=hn[:], in0=h1[:], scalar=rs[:, 0:1], in1=g2b[:],
                                                         op0=ALU.mult, op1=ALU.mult),
                   reads=[h1b, rsb, g2bb], writes=[hnb])
            hb, hbb = hbr.next()
            ctx.op(ACT, lambda e: e.copy(out=hb[:], in_=hn[:]), reads=[hnb], writes=[hbb])
            st[ti] = (hn, hnb, hb, hbb)

        def sc(ti):
            si, n, BW, b0, tt, r0, tg = tiles[ti]
            hn, hnb, hb, hbb = st[ti]
            for q4 in range(4):
                pt, ptb = ptr_.next()
                rd = [hnb, cb["ident_f"]]
                ctx.begin(PE, reads=rd, writes=[ptb])
                for k in range(4):
                    cc = q4 * 4 + k
                    ins = nc.tensor.transpose(out=pt[:, k * 128:(k + 1) * 128],
                                              in_=hn[:, cc * 128:(cc + 1) * 128], identity=c["ident_f"][:, :])
                ctx.end(PE, ins, reads=rd, writes=[ptb])
                ptv = pt[:, :].rearrange("p (c t) -> p c t", c=4)
                ctx.op(ACT, lambda e: e.copy(out=hT[:, q4 * 4:(q4 + 1) * 4, :], in_=ptv), reads=[ptb],
                       writes=[hTb])
            rd = [hTb, wrb]
            ctx.begin(PE, reads=rd, writes=[plogb])
            for cc in range(16):
                ins = nc.tensor.matmul(plog[:, 0:36], hT[:, cc, :], wr[:, cc, :], start=(cc == 0),
                                       stop=(cc == 15))
            ctx.end(PE, ins, reads=rd, writes=[plogb])
        def sd_(ti):
            si, n, BW, b0, tt, r0, tg = tiles[ti]
            dv(lambda e: e.tensor_copy(out=lg[:], in_=plog[:, 0:36]), [plogb], [lgb])
            dv(lambda e: e.reduce_max(out=gmax[:], in_=lg[:, 0:4], axis=AX.X), [lgb], [gmaxb])
            dv(lambda e: e.tensor_scalar(out=gm[:], in0=lg[:, 0:4], scalar1=gmax[:, 0:1], scalar2=None,
                                         op0=ALU.is_equal), [lgb, gmaxb], [gmb])
            dv(lambda e: e.tensor_scalar(out=ngmax[:], in0=gmax[:], scalar1=-1.0, scalar2=None, op0=ALU.mult),
               [gmaxb], [ngmaxb])
            ctx.op(ACT, lambda e: e.activation(out=eg[:], in_=lg[:, 0:4], func=AF.Exp, bias=ngmax[:, 0:1],
                                               scale=1.0, accum_out=se[:, 0:1]),
                   reads=[lgb, ngmaxb], writes=[egb, seb])
            dv(lambda e: e.reciprocal(out=pg[:], in_=se[:]), [seb], [pgb])
            dv(lambda e: e.tensor_scalar(out=pen[:], in0=gm[:], scalar1=BIG, scalar2=-BIG, op0=ALU.mult,
                                         op1=ALU.add), [gmb], [penb])
            for g in range(4):
                dv(lambda e: e.tensor_scalar(out=lm[:, g * 8:(g + 1) * 8], in0=lg[:, 4 + g * 8:4 + (g + 1) * 8],
                                             scalar1=pen[:, g:g + 1], scalar2=None, op0=ALU.add),
                   [lgb, penb], [lmb])
            dv(lambda e: e.reduce_max(out=m1[:], in_=lm[:], axis=AX.X), [lmb], [m1b])
            dv(lambda e: e.tensor_scalar(out=mk1[:], in0=lm[:], scalar1=m1[:, 0:1], scalar2=None,
                                         op0=ALU.is_equal), [lmb, m1b], [mk1b])
            dv(lambda e: e.scalar_tensor_tensor(out=lm2[:], in0=mk1[:], scalar=-BIG, in1=lm[:], op0=ALU.mult,
                                                op1=ALU.add), [mk1b, lmb], [lm2b])
            dv(lambda e: e.reduce_max(out=m2[:], in_=lm2[:], axis=AX.X), [lm2b], [m2b])
            dv(lambda e: e.tensor_scalar(out=mk2[:], in0=lm2[:], scalar1=m2[:, 0:1], scalar2=None,
                                         op0=ALU.is_equal), [lm2b, m2b], [mk2b])
            dv(lambda e: e.tensor_tensor(out=dd[:], in0=m1[:], in1=m2[:], op=ALU.subtract), [m1b, m2b], [ddb])
            ctx.op(ACT, lambda e: e.activation(out=w1[:], in_=dd[:], func=AF.Sigmoid), reads=[ddb], writes=[w1b])
            dv(lambda e: e.tensor_scalar(out=w2[:], in0=w1[:], scalar1=-1.0, scalar2=1.0, op0=ALU.mult,
                                         op1=ALU.add), [w1b], [w2b])
            dv(lambda e: e.tensor_tensor(out=G.cw1[:, tg:tg + 1], in0=pg[:], in1=w1[:], op=ALU.mult),
               [pgb, w1b], [G.cwb])
            dv(lambda e: e.tensor_tensor(out=G.cw2[:, tg:tg + 1], in0=pg[:], in1=w2[:], op=ALU.mult),
               [pgb, w2b], [G.cwb])
            dv(lambda e: e.tensor_tensor(out=sel[:], in0=mk1[:], in1=mk2[:], op=ALU.add), [mk1b, mk2b], [selb])

        def se_(ti):
            si, n, BW, b0, tt, r0, tg = tiles[ti]
            hn, hnb, hb, hbb = st.pop(ti)
            rd = [selb, selaccb, cb["tri_f"], cb["ones_f"]]
            ctx.begin(PE, reads=rd, writes=[prankb])
            nc.tensor.matmul(prank[:, 0:NE], c["tri_f"][:, :], sel[:, :], start=True, stop=False)
            ins = nc.tensor.matmul(prank[:, 0:NE], c["ones_f"][:, :], selacc[:, :], start=False, stop=True)
            ctx.end(PE, ins, reads=rd, writes=[prankb])
            dv(lambda e: e.tensor_copy(out=rk[:], in_=prank[:, 0:NE]), [prankb], [rkb])
            dv(lambda e: e.tensor_tensor(out=selacc[:], in0=selacc[:], in1=sel[:], op=ALU.add),
               [selb, selaccb], [selaccb])
            dv(lambda e: e.tensor_scalar(out=ov[:], in0=rk[:], scalar1=float(CAP), scalar2=1.0e6,
                                         op0=ALU.is_ge, op1=ALU.mult), [rkb], [ovb])
            dv(lambda e: e.tensor_tensor(out=sm[:], in0=rk[:], in1=c["iota_e"][:, :], op=ALU.add),
               [rkb, cb["iota_e"]], [smb])
            dv(lambda e: e.tensor_tensor(out=sm[:], in0=sm[:], in1=ov[:], op=ALU.add), [smb, ovb], [smb])
            dv(lambda e: e.tensor_tensor(out=tm[:], in0=sm[:], in1=mk1[:], op=ALU.mult), [smb, mk1b], [tmb])
            dv(lambda e: e.reduce_sum(out=sf1[:], in_=tm[:], axis=AX.X), [tmb], [sf1b])
            dv(lambda e: e.tensor_tensor(out=tm[:], in0=sm[:], in1=mk2[:], op=ALU.mult), [smb, mk2b], [tmb])
            dv(lambda e: e.reduce_sum(out=sf2[:], in_=tm[:], axis=AX.X), [tmb], [sf2b])
            dv(lambda e: e.tensor_copy(out=G.sl1[:, tg:tg + 1], in_=sf1[:]), [sf1b], [G.slb])
            dv(lambda e: e.tensor_copy(out=G.sl2[:, tg:tg + 1], in_=sf2[:]), [sf2b], [G.slb])
            for slt in (G.sl1, G.sl2):
                ctx.dma(POOL, lambda e: e.indirect_dma_start(
                    out=G.scr["Xs"][:, :], out_offset=bass.IndirectOffsetOnAxis(ap=slt[:, tg:tg + 1], axis=0),
                    in_=hb[:, :], in_offset=None, bounds_check=G.bcreg, oob_is_err=False),
                    sb=hbb, reads=[hbb, G.slb], writes=[G.dbuf["Xs"]])

        NTI = len(tiles)
        for it in range(NTI + 2):
            if 0 <= it - 2 < NTI:
                sd_(it - 2)
            if it < NTI:
                sa(it)
            if 0 <= it - 1 < NTI:
                sc(it - 1)
            if 0 <= it - 2 < NTI:
                se_(it - 2)
            if it < NTI:
                sb_(it)


def phase_M2(G):
    nc, ctx = G.nc, G.ctx
    PE, ACT, DVE, POOL, SP = ctx.PE, ctx.ACT, ctx.DVE, ctx.POOL, ctx.SP
    c, cb = G.c, G.cb
    NST = CAP // 128
    HW = CAP // 2
    with Scope(ctx) as S:
        ur = S.ring("wu", 9, [128, 4096], BF16)
        stgr = S.ring("stg", 3, [128, 4096], F32)
        castk = {"k": 0}

        def load_unit(dst_view, dstb, src_ap, cdim):
            stg, stgb = stgr.next()
            sv = stg[:, :].rearrange("p (c n) -> p c n", c=cdim)
            ctx.dma(SP, lambda e: e.dma_start(out=sv, in_=src_ap), sb=stgb, writes=[stgb])
            k = castk["k"] % 3
            castk["k"] += 1
            if k == 0:
                ctx.op(ACT, lambda e: e.copy(out=dst_view, in_=sv), reads=[stgb], writes=[dstb])
            elif k == 1:
                ctx.op(ACT, lambda e: e.copy(out=dst_view, in_=sv), reads=[stgb], writes=[dstb])
            else:
                ctx.op(DVE, lambda e: e.tensor_copy(out=dst_view, in_=sv), reads=[stgb], writes=[dstb])
        xrr = S.ring("xr", 2, [128, D], BF16)
        xtr = S.ring("XT", 2, [128, 16, CAP], BF16)
        hdr = S.ring("hid", 1, [128, 8, CAP], BF16)
        sgr = S.ring("sg", 2, [128, HW], F32)
        ysr = S.ring("ys", 2, [128, D], F32)
        ptxr = S.ring("ptx", 2, [128, 1024], BF16, psum=True)
        pgr = S.ring("pg", 2, [128, 512], F32, psum=True)
        pur = S.ring("pu", 2, [128, 512], F32, psum=True)
        pyr = S.ring("py", 2, [128, 512], F32, psum=True)
        ulist = []
        for ex_ in range(NE):
            for fp_ in range(4):
                ulist.append((ex_, "g", fp_))
                ulist.append((ex_, "u", fp_))
            for nb_ in range(4):
                ulist.append((ex_, "d", nb_))
        uslots = {}
        ustate = {"issued": 0}
        PF = 5

        def uget(k):
            while ustate["issued"] < min(len(ulist), k + PF + 1):
                kk = ustate["issued"]
                ex_, kind_, idx_ = ulist[kk]
                ut, utb = ur.next()
                if kind_ == "d":
                    view = ut[:, :].rearrange("p (c n) -> p c n", c=8)
                    src = G.din["w_ed"][ex_].rearrange("(c p) f -> p c f", p=128)[:, :, idx_ * 512:(idx_ + 1) * 512]
                    load_unit(view, utb, src, 8)
                else:
                    view = ut[:, :].rearrange("p (c n) -> p c n", c=16)
                    wsrc = G.din["w_eg" if kind_ == "g" else "w_eu"][ex_].rearrange("(c p) f -> p c f", p=128)
                    load_unit(view, utb, wsrc[:, :, idx_ * 256:(idx_ + 1) * 256], 16)
                uslots[kk] = (view, utb)
                ustate["issued"] += 1
            return uslots.pop(k)

        kev = 0
        for ex in range(NE):
            XT, XTb = xtr.next()
            for st in range(NST):
                xrw, xrwb = xrr.next()
                s0 = ex * CAP + st * 128
                ctx.dma(SP, lambda e: e.dma_start(out=xrw[:], in_=G.scr["Xs"][s0:s0 + 128, :]), sb=xrwb,
                        reads=[G.dbuf["Xs"]], writes=[xrwb])
                rd = [xrwb, cb["ident_b"]]
                for hf in range(2):
                    ptx, ptxb = ptxr.next()
                    ctx.begin(PE, reads=rd, writes=[ptxb])
                    for c8 in range(8):
                        cc = hf * 8 + c8
                        ins = nc.tensor.transpose(out=ptx[:, c8 * 128:(c8 + 1) * 128],
                                                  in_=xrw[:, cc * 128:(cc + 1) * 128], identity=c["ident_b"][:, :])
                    ctx.end(PE, ins, reads=rd, writes=[ptxb])
                    ptv = ptx[:, :].rearrange("p (c t) -> p c t", c=8)
                    if kev % 2 == 0:
                        ctx.op(ACT, lambda e: e.copy(out=XT[:, hf * 8:(hf + 1) * 8, st * 128:(st + 1) * 128], in_=ptv),
                               reads=[ptxb], writes=[XTb])
                    else:
                        ctx.op(DVE, lambda e: e.tensor_copy(out=XT[:, hf * 8:(hf + 1) * 8, st * 128:(st + 1) * 128],
                                                            in_=ptv), reads=[ptxb], writes=[XTb])
                    kev += 1
            hid, hidb = hdr.next()
            wg_r = G.din["w_eg"][ex].rearrange("(c p) f -> p c f", p=128)
            wu_r = G.din["w_eu"][ex].rearrange("(c p) f -> p c f", p=128)
            wd_r = G.din["w_ed"][ex].rearrange("(c p) f -> p c f", p=128)
            for fp in range(4):
                ugv, ugb = uget(ex * 12 + fp * 2)
                uuv, uub = uget(ex * 12 + fp * 2 + 1)
                for sub in range(2):
                    fcn = fp * 2 + sub
                    for half in range(2):
                        pg, pgb = pgr.next()
                        pu, pub = pur.next()
                        for (pp, ppb, wv, wvb) in ((pg, pgb, ugv, ugb), (pu, pub, uuv, uub)):
                            rd = [wvb, XTb]
                            ctx.begin(PE, reads=rd, writes=[ppb])
                            for cc in range(16):
                                ins = nc.tensor.matmul(pp[:, 0:HW], wv[:, cc, sub * 128:(sub + 1) * 128],
                                                       XT[:, cc, half * HW:(half + 1) * HW], start=(cc == 0),
                                                       stop=(cc == 15))
                            ctx.end(PE, ins, reads=rd, writes=[ppb])
                        sg, sgb = sgr.next()
                        ctx.op(ACT, lambda e: e.activation(out=sg[:, :], in_=pg[:, 0:HW], func=AF.Silu), reads=[pgb],
                               writes=[sgb])
                        ctx.op(DVE, lambda e: e.tensor_tensor(out=hid[:, fcn, half * HW:(half + 1) * HW], in0=pu[:, 0:HW],
                                                              in1=sg[:, :], op=ALU.mult),
                               reads=[pub, sgb], writes=[hidb])
            uds = []
            for nb in range(4):
                udv, udb = uget(ex * 12 + 8 + nb)
                uds.append((udv, udb))
            for st in range(NST):
                ys, ysb = ysr.next()
                for nb in range(4):
                    udv, udb = uds[nb]
                    py, pyb = pyr.next()
                    rd = [hidb, udb]
                    ctx.begin(PE, reads=rd, writes=[pyb])
                    for fc in range(8):
                        ins = nc.tensor.matmul(py[:, :], hid[:, fc, st * 128:(st + 1) * 128], udv[:, fc, :],
                                               start=(fc == 0), stop=(fc == 7))
                    ctx.end(PE, ins, reads=rd, writes=[pyb])
                    if nb % 2 == 0:
                        ctx.op(ACT, lambda e: e.copy(out=ys[:, nb * 512:(nb + 1) * 512], in_=py[:, :]), reads=[pyb],
                               writes=[ysb])
                    else:
                        ctx.op(DVE, lambda e: e.tensor_copy(out=ys[:, nb * 512:(nb + 1) * 512], in_=py[:, :]),
                               reads=[pyb], writes=[ysb])
                s0 = ex * CAP + st * 128
                ctx.dma(SP, lambda e: e.dma_start(out=G.scr["Ys"][s0:s0 + 128, :], in_=ys[:]), sb=ysb, reads=[ysb],
                        writes=[G.dbuf["Ys"]])


def phase_M3(G):
    nc, ctx = G.nc, G.ctx
    PE, ACT, DVE, POOL, SP = ctx.PE, ctx.ACT, ctx.DVE, ctx.POOL, ctx.SP
    with Scope(ctx) as S:
        gfb, gfbb = S.sb("gfb", [128, D], F32)
        ctx.dma(SP, lambda e: e.dma_start(out=gfb[:], in_=G.din["gfb"]), sb=gfbb, writes=[gfbb])
        epsc, epscb = S.sb("epsc", [128, 1], F32)
        ctx.op(DVE, lambda e: e.memset(epsc[:], EPS), writes=[epscb])
        y1r = S.ring("y1", 4, [128, D], F32)
        y2r = S.ring("y2", 4, [128, D], F32)
        h1r = S.ring("h1", 4, [128, D], F32)
        acr = S.ring("acc", 2, [128, D], F32)
        outr = S.ring("out", 3, [128, D], F32)
        junk, junkb = S.sb("junk", [128, D], BF16)
        ssr = S.ring("ss", 2, [128, 1], F32)
        sdr = S.ring("sd", 2, [128, 1], F32)
        rsr = S.ring("rs", 2, [128, 1], F32)
        tg = 0
        for si, n in enumerate(G.seq_ns):
            for r0 in range(0, n, 128):
                y1, y1b = y1r.next()
                y2, y2b = y2r.next()
                for (yt, ytb, slt) in ((y1, y1b, G.sl1), (y2, y2b, G.sl2)):
                    ctx.dma(POOL, lambda e: e.indirect_dma_start(
                        out=yt[:, :], out_offset=None, in_=G.scr["Ys"][:, :],
                        in_offset=bass.IndirectOffsetOnAxis(ap=slt[:, tg:tg + 1], axis=0),
                        bounds_check=G.bcreg, oob_is_err=False),
                        sb=ytb, reads=[G.dbuf["Ys"], G.slb], writes=[ytb])
                h1, h1b = h1r.next()
                ctx.dma(SP, lambda e: e.dma_start(out=h1[:], in_=G.scr[f"h1_{si}"][r0:r0 + 128, :]), sb=h1b,
                        reads=[G.dbuf[f"h1_{si}"]], writes=[h1b])
                acc, accb = acr.next()
                ctx.op(DVE, lambda e: e.scalar_tensor_tensor(out=acc[:], in0=y1[:], scalar=G.cw1[:, tg:tg + 1], in1=h1[:],
                                                             op0=ALU.mult, op1=ALU.add),
                       reads=[y1b, h1b, G.cwb], writes=[accb])
                ctx.op(DVE, lambda e: e.scalar_tensor_tensor(out=acc[:], in0=y2[:], scalar=G.cw2[:, tg:tg + 1], in1=acc[:],
                                                             op0=ALU.mult, op1=ALU.add),
                       reads=[y2b, accb, G.cwb], writes=[accb])
                ss, ssb = ssr.next()
                ctx.op(ACT, lambda e: e.activation(out=junk[:], in_=acc[:], func=AF.Square, accum_out=ss[:, 0:1]),
                       reads=[accb], writes=[junkb, ssb])
                sd, sdb = sdr.next()
                ctx.op(ACT, lambda e: e.activation(out=sd[:], in_=ss[:], func=AF.Sqrt, bias=epsc[:, 0:1], scale=1.0 / D),
                       reads=[ssb, epscb], writes=[sdb])
                rs, rsb = rsr.next()
                ctx.op(DVE, lambda e: e.reciprocal(out=rs[:], in_=sd[:]), reads=[sdb], writes=[rsb])
                ot, otb = outr.next()
                ctx.op(DVE, lambda e: e.scalar_tensor_tensor(out=ot[:], in0=acc[:], scalar=rs[:, 0:1], in1=gfb[:],
                                                             op0=ALU.mult, op1=ALU.mult),
                       reads=[accb, rsb, gfbb], writes=[otb])
                ctx.dma(SP, lambda e: e.dma_start(out=G.dout[f"y{si}"][r0:r0 + 128, :], in_=ot[:]), sb=otb, reads=[otb],
                        writes=[G.dbuf[f"y{si}"]])
                tg += 1


_PROG_CACHE = {}


def _rep(a):
    return np.ascontiguousarray(np.broadcast_to(np.asarray(a, np.float32).reshape(1, -1), (128, a.size)))


def kernel(x_prompt, x_sample, meta_tokens, mix_norm, w_in, q_gain, k_gain, w_attn_o, w_fourier_o, w_out,
           moe_norm, w_router_group, w_router_expert, w_expert_gate, w_expert_up, w_expert_down, final_norm):
    f = lambda a: np.ascontiguousarray(np.asarray(a, dtype=np.float32))
    x_prompt, x_sample = f(x_prompt), f(x_sample)
    ncores = 8
    seq_ns = [x_prompt.shape[1], x_sample.shape[1], x_sample.shape[1]]
    key = tuple(seq_ns)
    if key not in _PROG_CACHE:
        _PROG_CACHE[key] = build_program(seq_ns)
    nc, G = _PROG_CACHE[key]
    consts = host_consts(seq_ns)
    shared = {
        "meta": f(meta_tokens), "g1b": _rep(f(mix_norm)[0]), "g2b": _rep(f(moe_norm)[0]), "gfb": _rep(f(final_norm)),
        "qg": f(q_gain)[0].reshape(128, 1).copy(), "kg": f(k_gain)[0].reshape(128, 1).copy(),
        "w_in": f(w_in)[0], "w_ao": f(w_attn_o)[0], "w_fo": f(w_fourier_o)[0], "w_out": f(w_out)[0],
        "w_r": np.ascontiguousarray(np.concatenate([f(w_router_group)[0], f(w_router_expert)[0]], axis=1)),
        "w_eg": f(w_expert_gate)[0], "w_eu": f(w_expert_up)[0], "w_ed": f(w_expert_down)[0],
    }
    shared.update(consts)
    in_maps = []
    for cid in range(ncores):
        m = dict(shared)
        m["x0"] = x_prompt[cid]
        m["x1"] = x_sample[2 * cid]
        m["x2"] = x_sample[2 * cid + 1]
        in_maps.append({k: v for k, v in m.items() if k in G.din})
    res = run_bass_kernel_spmd(nc, in_maps, core_ids=list(range(ncores)))
    yp = np.stack([np.asarray(res.results[cid]["y0"], dtype=np.float32) for cid in range(ncores)], axis=0)
    ys = np.stack([np.asarray(res.results[cid][f"y{1 + j}"], dtype=np.float32)
                   for cid in range(ncores) for j in range(2)], axis=0)
    return (yp, ys)
```

```python
import numpy as np
import ml_dtypes
from contextlib import ExitStack
import concourse.bass as bass
import concourse.mybir as mybir
from concourse.bass_utils import run_bass_kernel_spmd

F32 = mybir.dt.float32
BF16 = mybir.dt.bfloat16
I32 = mybir.dt.int32
AF = mybir.ActivationFunctionType
ALU = mybir.AluOpType
AX = mybir.AxisListType

D = 2048
NH = 16
NKV = 4
HD = 128
FD = 1024
DE = 1024
NE = 32
NMETA = 16
GRID_W = 64
EPS = 1e-6
CAP = 640
NSLOT = NE * CAP


class Sem:
    def __init__(self, h, name):
        self.h = h
        self.name = name
        self.count = 0


class Buf:
    def __init__(self, name, acc=False):
        self.name = name
        self.w = {}
        self.r = {}
        self.acc = acc
        self.dsem = None


class EngW:
    def __init__(self, eng, sem, name):
        self.eng = eng
        self.sem = sem
        self.name = name
        self.seen = {}


class Ctx:
    def __init__(self, nc, es):
        self.nc = nc
        self.es = es
        self.uid = 0
        self.pool = []
        for i in range(88):
            h = es.enter_context(nc.semaphore(f"s{i}"))
            self.pool.append(Sem(h, f"s{i}"))
        self.allsems = list(self.pool)
        self.PE = EngW(nc.tensor, self.pool.pop(), "PE")
        self.ACT = EngW(nc.scalar, self.pool.pop(), "ACT")
        self.DVE = EngW(nc.vector, self.pool.pop(), "DVE")
        self.POOL = EngW(nc.gpsimd, self.pool.pop(), "POOL")
        self.SP = EngW(nc.sync, self.pool.pop(), "SP")
        self.engs = [self.PE, self.ACT, self.DVE, self.POOL, self.SP]

    def name(self, s):
        self.uid += 1
        return f"{s}_{self.uid}"

    def _need(self, reads, writes):
        need = {}

        def add(tok):
            k = id(tok[0])
            if k not in need or need[k][1] < tok[1]:
                need[k] = tok
        for b in reads:
            for t in b.w.values():
                add(t)
        for b in writes:
            for t in b.w.values():
                add(t)
            for t in b.r.values():
                add(t)
        return need

    def _wait(self, E, need):
        for k, (sem, val) in need.items():
            if sem is E.sem and E is self.PE:
                continue
            if E.seen.get(k, 0) >= val:
                continue
            E.eng.wait_ge(sem.h, val)
            E.seen[k] = val

    def _commit(self, tok, reads, writes):
        k = id(tok[0])
        for b in reads:
            b.r[k] = tok
        for b in writes:
            if b.acc:
                b.w[k] = tok
            else:
                b.w = {k: tok}
                b.r = {}

    def begin(self, E, reads=(), writes=()):
        self._wait(E, self._need(reads, writes))

    def end(self, E, ins, reads=(), writes=()):
        E.sem.count += 1
        ins.then_inc(E.sem.h, 1)
        self._commit((E.sem, E.sem.count), reads, writes)

    def op(self, E, fn, reads=(), writes=()):
        self.begin(E, reads, writes)
        ins = fn(E.eng)
        self.end(E, ins, reads, writes)

    def dma(self, E, fn, sb, reads=(), writes=()):
        if sb.dsem is None:
            sb.dsem = self.pool.pop()
        self.begin(E, reads, writes)
        ins = fn(E.eng)
        sb.dsem.count += 16
        ins.then_inc(sb.dsem.h, 16)
        self._commit((sb.dsem, sb.dsem.count), reads, writes)

    def release(self, bufs):
        for b in bufs:
            if b.dsem is not None:
                self.pool.append(b.dsem)
                b.dsem = None

    def barrier(self):
        for E in self.engs:
            need = {}
            for s in self.allsems:
                if s.count > 0:
                    need[id(s)] = (s, s.count)
            for k, (sem, val) in need.items():
                if E.seen.get(k, 0) >= val:
                    continue
                E.eng.wait_ge(sem.h, val)
                E.seen[k] = val


class Scope:
    def __init__(self, ctx):
        self.ctx = ctx
        self.es = ExitStack()
        self.bufs = []

    def __enter__(self):
        self.es.__enter__()
        return self

    def __exit__(self, *a):
        self.ctx.barrier()
        self.ctx.release(self.bufs)
        return self.es.__exit__(*a)

    def sb(self, name, shape, dt):
        t = self.es.enter_context(self.ctx.nc.sbuf_tensor(self.ctx.name(name), shape, dt))
        b = Buf(name)
        self.bufs.append(b)
        return t, b

    def ps(self, name, shape, dt):
        t = self.es.enter_context(self.ctx.nc.psum_tensor(self.ctx.name(name), shape, dt))
        b = Buf(name)
        self.bufs.append(b)
        return t, b

    def ring(self, name, n, shape, dt, psum=False):
        return Ring([(self.ps if psum else self.sb)(f"{name}{i}", shape, dt) for i in range(n)])


class Ring:
    def __init__(self, items):
        self.items = items
        self.i = 0

    def next(self):
        it = self.items[self.i % len(self.items)]
        self.i += 1
        return it


def _bf(a):
    return np.ascontiguousarray(a.astype(ml_dtypes.bfloat16))


def rope_tables(n):
    half = 32
    inv = (10000.0 ** (-np.arange(half, dtype=np.float32) / half)).astype(np.float32)
    t = np.arange(n)
    row = (t // GRID_W).astype(np.float32)
    col = (t % GRID_W).astype(np.float32)
    i = np.arange(HD)
    axis = i // 64
    f = i % 32
    pos = np.where(axis[:, None] == 0, row[None, :], col[None, :]).astype(np.float32)
    ang = (pos * inv[f][:, None]).astype(np.float32)
    return np.cos(ang).astype(np.float32), np.sin(ang).astype(np.float32)


def rot_matrix_T():
    R = np.zeros((HD, HD), np.float32)
    for i in range(HD):
        if (i % 64) < 32:
            R[i, i + 32] = -1.0
        else:
            R[i, i - 32] = 1.0
    return R.T.copy()


def chan_dft_table():
    c = np.arange(256)[:, None].astype(np.float64)
    cp = np.arange(256)[None, :].astype(np.float64)
    b = 2 * np.pi * ((c * cp) % 256) / 256.0
    tab = np.concatenate([np.cos(b) / 16.0, np.sin(b) / 16.0], axis=1)
    return tab.reshape(2, 128, 512).transpose(1, 0, 2).copy()


def pos_dft_table(n, lb):
    L = n + NMETA
    nt = 1 + n // 128
    lidx = np.zeros((128, nt), np.int64)
    valid = np.zeros((128, nt), bool)
    lidx[:16, 0] = np.arange(16)
    valid[:16, 0] = True
    for t in range(1, nt):
        lidx[:, t] = 16 + (t - 1) * 128 + np.arange(128)
        valid[:, t] = True
    out = np.zeros((n // lb, 128, nt, 2, lb), ml_dtypes.bfloat16)
    s = 1.0 / np.sqrt(L)
    for j in range(n // lb):
        lp = 16 + j * lb + np.arange(lb)
        a = 2 * np.pi * ((lidx[:, :, None] * lp[None, None, :]) % L) / L
        c = np.cos(a) * s * valid[:, :, None]
        sn = -np.sin(a) * s * valid[:, :, None]
        out[j, :, :, 0, :] = c.astype(ml_dtypes.bfloat16)
        out[j, :, :, 1, :] = sn.astype(ml_dtypes.bfloat16)
    return out


_CONST_CACHE = {}


def host_consts(seq_ns):
    key = tuple(seq_ns)
    if key in _CONST_CACHE:
        return _CONST_CACHE[key]
    c = {}
    c["ident_b"] = _bf(np.eye(128, dtype=np.float32))
    c["ident_f"] = np.eye(128, dtype=np.float32)
    c["ones_b"] = _bf(np.ones((128, 128), np.float32))
    c["onesm_b"] = _bf(np.full((128, 128), 1.0 / 128, np.float32))
    c["ones_f"] = np.ones((128, 128), np.float32)
    tri = (np.arange(128)[:, None] < np.arange(128)[None, :]).astype(np.float32)
    c["tri_f"] = tri
    c["rotT_b"] = _bf(rot_matrix_T())
    c["cc_b"] = _bf(chan_dft_table())
    c["iota_e"] = np.tile(np.arange(NE, dtype=np.float32)[None, :] * CAP, (128, 1))
    for n in sorted(set(seq_ns)):
        cs, sn = rope_tables(n)
        c[f"ropec_{n}"] = cs
        c[f"ropes_{n}"] = sn
        c[f"dft_{n}"] = pos_dft_table(n, lb_for(n))
    _CONST_CACHE[key] = c
    return c


def lb_for(n):
    return 256 if n > 2048 else min(512, n)


def gb_for(n):
    return 2 if n > 2048 else 4


class Prog:
    pass


def build_program(seq_ns, debug=False, upto=99):
    nc = bass.Bass("TRN2", target_bir_lowering=False)
    G = Prog()
    G.nc = nc
    G.seq_ns = list(seq_ns)
    G.debug = debug
    G.din = {}
    G.dout = {}
    G.scr = {}
    G.dbuf = {}

    def din(name, shape, dt):
        G.din[name] = nc.dram_tensor(name, list(shape), dt, kind="ExternalInput").ap()
        return G.din[name]

    def dscr(name, shape, dt, dbg=True):
        kind = "ExternalOutput" if (debug and dbg) else "Internal"
        G.scr[name] = nc.dram_tensor(name, list(shape), dt, kind=kind).ap()
        G.dbuf[name] = Buf(name, acc=True)
        return G.scr[name]

    for i, n in enumerate(seq_ns):
        din(f"x{i}", [n, D], F32)
        G.dout[f"y{i}"] = nc.dram_tensor(f"y{i}", [n, D], F32, kind="ExternalOutput").ap()
        G.dbuf[f"y{i}"] = Buf(f"y{i}", acc=True)
    din("meta", [NMETA, D], F32)
    din("g1b", [128, D], F32)
    din("g2b", [128, D], F32)
    din("gfb", [128, D], F32)
    din("qg", [128, 1], F32)
    din("kg", [128, 1], F32)
    din("w_in", [D, 8192], F32)
    if upto >= 4:
        din("w_ao", [D, D], F32)
        din("w_fo", [FD, D], F32)
    if upto >= 5:
        din("w_out", [D, D], F32)
        din("w_r", [D, 36], F32)
    if upto >= 6:
        din("w_eg", [NE, D, DE], F32)
        din("w_eu", [NE, D, DE], F32)
        din("w_ed", [NE, DE, D], F32)
    din("ident_b", [128, 128], BF16)
    din("ident_f", [128, 128], F32)
    din("ones_b", [128, 128], BF16)
    din("onesm_b", [128, 128], BF16)
    din("ones_f", [128, 128], F32)
    din("tri_f", [128, 128], F32)
    din("rotT_b", [128, 128], BF16)
    din("cc_b", [128, 2, 512], BF16)
    din("iota_e", [128, NE], F32)
    for n in sorted(set(seq_ns)):
        din(f"ropec_{n}", [128, n], F32)
        din(f"ropes_{n}", [128, n], F32)
        lb = lb_for(n)
        din(f"dft_{n}", [n // lb, 128, 1 + n // 128, 2, lb], BF16)

    dscr("kTm", [NKV, 128, NMETA], BF16)
    dscr("vm", [NMETA, 512], BF16)
    dscr("ABm", [NMETA, 2048], BF16)
    for i, n in enumerate(seq_ns):
        dscr(f"qT{i}", [NH, 128, n], BF16)
        dscr(f"kT{i}", [NKV, 128, n], BF16)
        dscr(f"v{i}", [n, 512], BF16)
        dscr(f"AB{i}", [n, 2048], BF16)
        dscr(f"sga{i}", [D, n], BF16)
        dscr(f"sgf{i}", [D, n], BF16)
        dscr(f"ZT{i}", [FD, n], BF16)
        dscr(f"attnT{i}", [D, n], BF16)
        dscr(f"mT{i}", [D, n], BF16)
        dscr(f"h1_{i}", [n, D], F32)
    dscr("Xs", [NSLOT, D], BF16, dbg=False)
    dscr("Ys", [NSLOT, D], F32, dbg=False)

    with ExitStack() as es:
        ctx = Ctx(nc, es)
        G.ctx = ctx
        G.bcreg = es.enter_context(nc.gpsimd.register("bcreg"))
        nc.gpsimd.reg_mov(G.bcreg, NSLOT - 1)
        with Scope(ctx) as CS:
            G.CS = CS
            load_consts(G)
            NT = sum(seq_ns) // 128
            G.cw1, G.cwb = CS.sb("cw1", [128, NT], F32)
            G.cw2, _ = CS.sb("cw2", [128, NT], F32)
            G.sl1, G.slb = CS.sb("sl1", [128, NT], I32)
            G.sl2, _ = CS.sb("sl2", [128, NT], I32)
            precast(G)
            G.marks = []
            mark = lambda nm: G.marks.append((nm, ctx.PE.sem.count, ctx.ACT.sem.count, ctx.DVE.sem.count))
            mark("A")
            if upto >= 1:
                phase_A(G)
            if upto >= 2:
                for i in range(len(seq_ns)):
                    mark(f"F{i}")
                    phase_F(G, i)
            if upto >= 3:
                for i in range(len(seq_ns)):
                    mark(f"At{i}")
                    phase_At(G, i)
            if upto >= 4:
                mark("G1")
                phase_G1(G)
            if upto >= 5:
                mark("G2")
                phase_G2(G)
            if upto >= 6:
                mark("M2")
                phase_M2(G)
            if upto >= 7:
                mark("M3")
                phase_M3(G)
            mark("end")
            ctx.barrier()
    return nc, G


def precast(G):
    nc, ctx = G.nc, G.ctx
    POOL = ctx.POOL
    G.wb = {}
    G.wbb = {}

    def one(name, src, rows, cols, upto_ok):
        if not upto_ok:
            return
        dst = nc.dram_tensor(name + "_bf", [rows, cols], BF16).ap()
        G.wb[name] = dst
        nblk = cols // 512
        grp = 4
        for g0 in range(0, nblk, grp):
            holder = Buf(f"{name}_h{g0}")
            G.CS.bufs.append(holder)
            db = Buf(f"{name}_d{g0}", acc=True)
            for b in range(g0, min(nblk, g0 + grp)):
                G.wbb[(name, b)] = db
                ctx.dma(POOL, lambda e: e.dma_start(out=dst[:, b * 512:(b + 1) * 512],
                                                    in_=src[:, b * 512:(b + 1) * 512]), sb=holder, writes=[db])
    one("w_in", G.din["w_in"], D, 8192, True)
    one("w_ao", G.din.get("w_ao"), D, D, "w_ao" in G.din)
    one("w_fo", G.din.get("w_fo"), FD, D, "w_fo" in G.din)
    one("w_out", G.din.get("w_out"), D, D, "w_out" in G.din)


def load_consts(G):
    ctx, CS = G.ctx, G.CS
    G.c = {}
    G.cb = {}
    for name, shape, dt in [("ident_b", [128, 128], BF16), ("ident_f", [128, 128], F32),
                            ("ones_b", [128, 128], BF16), ("onesm_b", [128, 128], BF16),
                            ("ones_f", [128, 128], F32), ("tri_f", [128, 128], F32),
                            ("rotT_b", [128, 128], BF16), ("cc_b", [128, 2, 512], BF16),
                            ("iota_e", [128, NE], F32), ("qg", [128, 1], F32), ("kg", [128, 1], F32)]:
        t, b = CS.sb(name, shape, dt)
        G.c[name], G.cb[name] = t, b
        src = G.din[name]
        ctx.dma(ctx.SP, lambda e, t=t, src=src: e.dma_start(out=t[:], in_=src), sb=b, writes=[b])


def phase_A(G):
    nc, ctx = G.nc, G.ctx
    PE, ACT, DVE, POOL, SP = ctx.PE, ctx.ACT, ctx.DVE, ctx.POOL, ctx.SP
    c, cb = G.c, G.cb
    SBW = min(1024, min(G.seq_ns))
    w_in_r = G.wb["w_in"].rearrange("(c p) n -> p c n", p=128)
    with Scope(ctx) as S:
        ntmax = SBW // 128
        hn_slots = []
        for i in range(2):
            t, _ = S.sb(f"hnT{i}", [128, 16, SBW], BF16)
            hn_slots.append((t, [Buf(f"hnT{i}_{j}") for j in range(ntmax)]))
        uT, _ = S.sb("uT", [128, 8, SBW], BF16)
        uTb = [Buf(f"uT{i}") for i in range(max(1, SBW // 512))]
        wring = S.ring("w", 3, [128, 16, 512], BF16)
        xring = S.ring("x", 2, [128, D], F32)
        xnring = S.ring("xn", 2, [128, D], BF16)
        g1b, g1bb = S.sb("g1b", [128, D], F32)
        ctx.dma(SP, lambda e: e.dma_start(out=g1b[:], in_=G.din["g1b"]), sb=g1bb, writes=[g1bb])
        ropeCr = S.ring("ropeC", 1, [128, SBW], F32)
        ropeSr = S.ring("ropeS", 1, [128, SBW], F32)
        ssr = S.ring("ss", 2, [128, 1], F32)
        sdr = S.ring("sd", 2, [128, 1], F32)
        rsr = S.ring("rs", 2, [128, 1], F32)
        sqr = S.ring("sq", 2, [128, 512], BF16)
        sdtr = S.ring("sdt", 2, [128, 512], F32)
        qnr = S.ring("qn", 2, [128, 512], BF16)
        t1r = S.ring("t1", 2, [128, 512], F32)
        t2r = S.ring("t2", 2, [128, 512], F32)
        qfr = S.ring("qf", 2, [128, 512], BF16)
        ogr = S.ring("og", 2, [128, 512], BF16)
        abr = S.ring("ab", 2, [128, 4, 512], BF16)
        pt, ptb = S.ps("pt", [128, 2048], BF16)
        pjr = S.ring("pj", 3, [128, 512], F32, psum=True)
        pm, pmb = S.ps("pm", [128, 512], F32)
        pr, prb = S.ps("pr", [128, 512], F32)
        pdr = S.ring("pd", 1, [128, 512], F32, psum=True)
        epsc, epscb = S.sb("epsc", [128, 1], F32)
        c["epsc"], cb["epsc"] = epsc, epscb
        ctx.op(DVE, lambda e: e.memset(epsc[:], EPS), writes=[epscb])

        class SB:
            pass
        sbs = []

        def mk(src, W, meta, si, t0):
            o = SB()
            o.src, o.W, o.meta, o.si, o.t0 = src, W, meta, si, t0
            o.TW = min(128, W)
            o.ntile = W // o.TW
            o.BW = min(512, W)
            o.nblk = W // o.BW
            o.tpb = o.BW // o.TW
            o.hnT, o.hnTb = hn_slots[len(sbs) % 2]
            o.wbs = [wb for wb in range(16) if not (meta and (2 <= wb < 6 or wb >= 8))]
            o.xn = {}
            sbs.append(o)
        mk(G.din["meta"], NMETA, True, -1, 0)
        for si, n in enumerate(G.seq_ns):
            for t0 in range(0, n, SBW):
                mk(G.din[f"x{si}"][t0:t0 + SBW, :], SBW, False, si, t0)

        wtasks = [(o, wb) for o in sbs for wb in o.wbs]
        wslots = {}
        wstate = {"issued": 0}

        def wget(k):
            while wstate["issued"] < min(len(wtasks), k + 3):
                kk = wstate["issued"]
                wt, wbuf = wring.next()
                wb = wtasks[kk][1]
                ctx.dma(SP, lambda e: e.dma_start(out=wt[:], in_=w_in_r[:, :, wb * 512:(wb + 1) * 512]),
                        sb=wbuf, reads=[G.wbb[("w_in", wb)]], writes=[wbuf])
                wslots[kk] = (wt, wbuf)
                wstate["issued"] += 1
            return wslots.pop(k)

        def a1_prep(o, ti):
            TW = o.TW
            xt, xb = xring.next()
            ctx.dma(SP, lambda e: e.dma_start(out=xt[0:TW, :], in_=o.src[ti * TW:(ti + 1) * TW, :]),
                    sb=xb, writes=[xb])
            ss, ssb = ssr.next()
            xn, xnb = xnring.next()
            ctx.op(ACT, lambda e: e.activation(out=xn[0:TW, :], in_=xt[0:TW, :], func=AF.Square,
                                               accum_out=ss[0:TW, 0:1]),
                   reads=[xb], writes=[xnb, ssb])
            sd, sdb = sdr.next()
            ctx.op(ACT, lambda e: e.activation(out=sd[0:TW, :], in_=ss[0:TW, :], func=AF.Sqrt,
                                               bias=epsc[0:TW, 0:1], scale=1.0 / D),
                   reads=[ssb, epscb], writes=[sdb])
            rs, rsb = rsr.next()
            ctx.op(DVE, lambda e: e.reciprocal(out=rs[0:TW, :], in_=sd[0:TW, :]), reads=[sdb], writes=[rsb])
            ctx.op(DVE, lambda e: e.scalar_tensor_tensor(out=xn[0:TW, :], in0=xt[0:TW, :], scalar=rs[0:TW, 0:1],
                                                         in1=g1b[0:TW, :], op0=ALU.mult, op1=ALU.mult),
                   reads=[xb, rsb, g1bb], writes=[xnb])
            o.xn[ti] = (xn, xnb)

        def a1_trans(o, ti):
            TW = o.TW
            xn, xnb = o.xn.pop(ti)
            ctx.begin(PE, reads=[xnb, cb["ident_b"]], writes=[ptb])
            for cc in range(16):
                ins = nc.tensor.transpose(out=pt[:, cc * 128:cc * 128 + TW], in_=xn[0:TW, cc * 128:(cc + 1) * 128],
                                          identity=c["ident_b"][0:TW, 0:TW])
            ctx.end(PE, ins, reads=[xnb, cb["ident_b"]], writes=[ptb])
            ptv = pt[:, :].rearrange("p (c t) -> p c t", c=16)
            ctx.op(ACT, lambda e: e.copy(out=o.hnT[:, :, ti * TW:(ti + 1) * TW], in_=ptv[:, :, 0:TW]),
                   reads=[ptb], writes=[o.hnTb[ti]])

        def a1_steps(o):
            steps = []
            for k in range(o.ntile + 1):
                def st(k=k):
                    if k < o.ntile:
                        a1_prep(o, k)
                    if k >= 1:
                        a1_trans(o, k - 1)
                steps.append(st)
            return steps

        def a2(o, wk0, hooks):
            meta, si, t0, TW, BW, W = o.meta, o.si, o.t0, o.TW, o.BW, o.W
            ntile, nblk, tpb = o.ntile, o.nblk, o.tpb
            hnT, hnTb = o.hnT, o.hnTb
            if not meta:
                n = G.seq_ns[si]
                ropeC, ropeCb = ropeCr.next()
                ropeS, ropeSb = ropeSr.next()
                ctx.dma(SP, lambda e: e.dma_start(out=ropeC[:, 0:W], in_=G.din[f"ropec_{n}"][:, t0:t0 + W]),
                        sb=ropeCb, writes=[ropeCb])
                ctx.dma(SP, lambda e: e.dma_start(out=ropeS[:, 0:W], in_=G.din[f"ropes_{n}"][:, t0:t0 + W]),
                        sb=ropeSb, writes=[ropeSb])

            q1, q2 = [], []

            def tick():
                run = list(q2)
                q2.clear()
                for f in run:
                    f()
                run = list(q1)
                q1.clear()
                for f in run:
                    f()

            def qk_pipeline(ps, psb, gain, gainb, dst_fn, blk):
                sq, sqb = sqr.next()
                ctx.op(ACT, lambda e: e.activation(out=sq[:, 0:BW], in_=ps[:, 0:BW], func=AF.Square),
                       reads=[psb], writes=[sqb])

                def stage1():
                    ctx.begin(PE, reads=[sqb, cb["onesm_b"]], writes=[pmb])
                    ins = nc.tensor.matmul(pm[:, 0:BW], c["onesm_b"][:, :], sq[:, 0:BW], start=True, stop=True)
                    ctx.end(PE, ins, reads=[sqb, cb["onesm_b"]], writes=[pmb])
                    sdt, sdtb = sdtr.next()
                    ctx.op(ACT, lambda e: e.activation(out=sdt[:, 0:BW], in_=pm[:, 0:BW], func=AF.Sqrt,
                                                       bias=epsc[:, 0:1], scale=1.0),
                           reads=[pmb, epscb], writes=[sdtb])
                    rst, rstb = sdt, sdtb
                    ctx.op(DVE, lambda e: e.reciprocal(out=rst[:, 0:BW], in_=sdt[:, 0:BW]), reads=[sdtb],
                           writes=[rstb])
                    qn, qnb = qnr.next()
                    ctx.op(DVE, lambda e: e.scalar_tensor_tensor(out=qn[:, 0:BW], in0=ps[:, 0:BW],
                                                                 scalar=gain[:, 0:1], in1=rst[:, 0:BW],
                                                                 op0=ALU.mult, op1=ALU.mult),
                           reads=[psb, gainb, rstb], writes=[qnb])
                    if meta:
                        dst_fn(qn, qnb)
                        return

                    def stage2():
                        ctx.begin(PE, reads=[qnb, cb["rotT_b"]], writes=[prb])
                        ins = nc.tensor.matmul(pr[:, 0:BW], c["rotT_b"][:, :], qn[:, 0:BW], start=True, stop=True)
                        ctx.end(PE, ins, reads=[qnb, cb["rotT_b"]], writes=[prb])
                        t1, t1b = t1r.next()
                        ctx.op(POOL, lambda e: e.tensor_tensor(out=t1[:, 0:BW], in0=qn[:, 0:BW],
                                                               in1=ropeC[:, blk * BW:(blk + 1) * BW], op=ALU.mult),
                               reads=[qnb, ropeCb], writes=[t1b])
                        t2, t2b = t2r.next()
                        ctx.op(DVE, lambda e: e.tensor_tensor(out=t2[:, 0:BW], in0=pr[:, 0:BW],
                                                              in1=ropeS[:, blk * BW:(blk + 1) * BW], op=ALU.mult),
                               reads=[prb, ropeSb], writes=[t2b])
                        qf, qfb = qfr.next()
                        ctx.op(POOL, lambda e: e.tensor_tensor(out=qf[:, 0:BW], in0=t1[:, 0:BW], in1=t2[:, 0:BW],
                                                               op=ALU.add),
                               reads=[t1b, t2b], writes=[qfb])
                        dst_fn(qf, qfb)
                    q2.append(stage2)
                q1.append(stage1)

            for wi, wb in enumerate(o.wbs):
                kind = "u" if wb < 2 else "q" if wb < 6 else "k" if wb == 6 else "v" if wb == 7 else \
                    "ga" if wb < 12 else "gf"
                wt, wbuf = wget(wk0 + wi)
                if kind == "v":
                    for ti in range(ntile):
                        ps, psb = pjr.next()
                        rd = [hnTb[ti], wbuf]
                        ctx.begin(PE, reads=rd, writes=[psb])
                        for cc in range(16):
                            ins = nc.tensor.matmul(ps[0:TW, :], hnT[:, cc, ti * TW:(ti + 1) * TW], wt[:, cc, :],
                                                   start=(cc == 0), stop=(cc == 15))
                        ctx.end(PE, ins, reads=rd, writes=[psb])
                        tick()
                        st, stb = ogr.next()
                        ctx.op(ACT, lambda e: e.copy(out=st[0:TW, :], in_=ps[0:TW, :]), reads=[psb], writes=[stb])
                        if meta:
                            dst, dname = G.scr["vm"][:, :], "vm"
                        else:
                            dst, dname = G.scr[f"v{si}"][t0 + ti * TW:t0 + (ti + 1) * TW, :], f"v{si}"
                        ctx.dma(SP, lambda e: e.dma_start(out=dst, in_=st[0:TW, :]), sb=stb, reads=[stb],
                                writes=[G.dbuf[dname]])
                    hooks(wi)
                    continue
                for blk in range(nblk):
                    for ch in range(4):
                        ps, psb = pjr.next()
                        rd = [hnTb[blk * tpb + j] for j in range(tpb)] + [wbuf]
                        ctx.begin(PE, reads=rd, writes=[psb])
                        for cc in range(16):
                            ins = nc.tensor.matmul(ps[:, 0:BW], wt[:, cc, ch * 128:(ch + 1) * 128],
                                                   hnT[:, cc, blk * BW:(blk + 1) * BW],
                                                   start=(cc == 0), stop=(cc == 15))
                        ctx.end(PE, ins, reads=rd, writes=[psb])
                        tick()
                        c0 = t0 + blk * BW
                        if kind == "u":
                            uc = wb * 4 + ch
                            ctx.op(ACT, lambda e: e.copy(out=uT[:, uc, blk * BW:(blk + 1) * BW], in_=ps[:, 0:BW]),
                                   reads=[psb], writes=[uTb[blk]])
                        elif kind in ("q", "k"):
                            hh = (wb - 2) * 4 + ch if kind == "q" else ch
                            if kind == "q":
                                dst, dname = G.scr[f"qT{si}"][hh, :, c0:c0 + BW], f"qT{si}"
                            elif meta:
                                dst, dname = G.scr["kTm"][hh, :, :], "kTm"
                            else:
                                dst, dname = G.scr[f"kT{si}"][hh, :, c0:c0 + BW], f"kT{si}"

                            def dst_fn(tl, tlb, dst=dst, dname=dname):
                                ctx.dma(SP, lambda e: e.dma_start(out=dst, in_=tl[:, 0:BW]), sb=tlb, reads=[tlb],
                                        writes=[G.dbuf[dname]])
                            gname = "qg" if kind == "q" else "kg"
                            qk_pipeline(ps, psb, c[gname], cb[gname], dst_fn, blk)
                        else:
                            st, stb = ogr.next()
                            ctx.op(ACT, lambda e: e.activation(out=st[:, 0:BW], in_=ps[:, 0:BW], func=AF.Sigmoid),
                                   reads=[psb], writes=[stb])
                            base = (wb - 8) * 512 if kind == "ga" else (wb - 12) * 512
                            dname = f"sga{si}" if kind == "ga" else f"sgf{si}"
                            dst = G.scr[dname][base + ch * 128:base + (ch + 1) * 128, c0:c0 + BW]
                            ctx.dma(SP, lambda e: e.dma_start(out=dst, in_=st[:, 0:BW]), sb=stb, reads=[stb],
                                    writes=[G.dbuf[dname]])
                if kind == "u" and wb == 1:
                    for ti in range(ntile):
                        abt, abtb = abr.next()
                        for g in range(4):
                            pd, pdb = pdr.next()
                            rd = [uTb[ti // tpb], cb["cc_b"]]
                            ctx.begin(PE, reads=rd, writes=[pdb])
                            for kc in range(2):
                                ins = nc.tensor.matmul(pd[0:TW, :], uT[:, 2 * g + kc, ti * TW:(ti + 1) * TW],
                                                       c["cc_b"][:, kc, :], start=(kc == 0), stop=(kc == 1))
                            ctx.end(PE, ins, reads=rd, writes=[pdb])
                            ctx.op(DVE, lambda e: e.tensor_copy(out=abt[0:TW, g, :], in_=pd[0:TW, :]),
                                   reads=[pdb], writes=[abtb])
                        if meta:
                            dst, dname = G.scr["ABm"][:, :], "ABm"
                        else:
                            dst, dname = G.scr[f"AB{si}"][t0 + ti * TW:t0 + (ti + 1) * TW, :], f"AB{si}"
                        dstv = dst.rearrange("t (g f) -> t g f", g=4)
                        ctx.dma(SP, lambda e: e.dma_start(out=dstv, in_=abt[0:TW, :, :]), sb=abtb, reads=[abtb],
                                writes=[G.dbuf[dname]])
                hooks(wi)
            tick()
            tick()

        for st in a1_steps(sbs[0]):
            st()
        wk = 0
        for i, o in enumerate(sbs):
            nxt = a1_steps(sbs[i + 1]) if i + 1 < len(sbs) else []
            pos = {"k": 0}

            def hooks(wi, nxt=nxt, pos=pos):
                if wi >= 1 and pos["k"] < len(nxt):
                    nxt[pos["k"]]()
                    pos["k"] += 1
            a2(o, wk, hooks)
            wk += len(o.wbs)
            while pos["k"] < len(nxt):
                nxt[pos["k"]]()
                pos["k"] += 1


def phase_F(G, si):
    nc, ctx = G.nc, G.ctx
    PE, ACT, DVE, POOL, SP = ctx.PE, ctx.ACT, ctx.DVE, ctx.POOL, ctx.SP
    n = G.seq_ns[si]
    LB, GB = lb_for(n), gb_for(n)
    nt = 1 + n // 128
    ABd = G.scr[f"AB{si}"].rearrange("(t p) f -> p t f", p=128)
    dft = G.din[f"dft_{n}"]
    with Scope(ctx) as S:
        ABs, ABsb = S.sb("ABs", [128, nt, GB * 512], BF16)
        csr = S.ring("cs", 3, [128, nt, 2, LB], BF16)
        ztr = S.ring("zt", 4, [128, LB], BF16)
        pzr = S.ring("pz", 4, [128, 512], F32, psum=True)
        k_ev = 0
        for gs in range(0, 4, GB):
            for gi in range(GB):
                g = gs + gi
                for ta in range(0, nt - 1, 8):
                    tb_ = min(nt - 1, ta + 8)
                    ctx.dma(SP, lambda e: e.dma_start(out=ABs[:, 1 + ta:1 + tb_, gi * 512:(gi + 1) * 512],
                                                      in_=ABd[:, ta:tb_, g * 512:(g + 1) * 512]),
                            sb=ABsb, reads=[G.dbuf[f"AB{si}"]], writes=[ABsb])
                ctx.dma(SP, lambda e: e.dma_start(out=ABs[0:NMETA, 0, gi * 512:(gi + 1) * 512],
                                                  in_=G.scr["ABm"][:, g * 512:(g + 1) * 512]),
                        sb=ABsb, reads=[G.dbuf["ABm"]], writes=[ABsb])
            for j in range(n // LB):
                cs, csb = csr.next()
                ctx.dma(SP, lambda e: e.dma_start(out=cs[:], in_=dft[j]), sb=csb, writes=[csb])
                for gi in range(GB):
                    g = gs + gi
                    for cc in range(2):
                        ps, psb = pzr.next()
                        rd = [ABsb, csb]
                        ctx.begin(PE, reads=rd, writes=[psb])
                        k = 0
                        for t in range(nt):
                            rows = NMETA if t == 0 else 128
                            for ab in range(2):
                                col = gi * 512 + ab * 256 + cc * 128
                                ins = nc.tensor.matmul(ps[:, 0:LB], ABs[0:rows, t, col:col + 128],
                                                       cs[0:rows, t, ab, :], start=(k == 0),
                                                       stop=(k == 2 * nt - 1))
                                k += 1
                        ctx.end(PE, ins, reads=rd, writes=[psb])
                        zt, ztb = ztr.next()
                        if k_ev % 2 == 0:
                            ctx.op(ACT, lambda e: e.copy(out=zt[:, 0:LB], in_=ps[:, 0:LB]), reads=[psb], writes=[ztb])
                        else:
                            ctx.op(DVE, lambda e: e.tensor_copy(out=zt[:, 0:LB], in_=ps[:, 0:LB]), reads=[psb],
                                   writes=[ztb])
                        k_ev += 1
                        r0 = g * 256 + cc * 128
                        ctx.dma(SP, lambda e: e.dma_start(out=G.scr[f"ZT{si}"][r0:r0 + 128, j * LB:(j + 1) * LB],
                                                          in_=zt[:, 0:LB]),
                                sb=ztb, reads=[ztb], writes=[G.dbuf[f"ZT{si}"]])


def phase_At(G, si):
    nc, ctx = G.nc, G.ctx
    PE, ACT, DVE, POOL, SP = ctx.PE, ctx.ACT, ctx.DVE, ctx.POOL, ctx.SP
    c, cb = G.c, G.cb
    n = G.seq_ns[si]
    L = n + NMETA
    nt = 1 + n // 128
    npair = (nt - 1) // 2
    QB = min(512, n)
    vd = G.scr[f"v{si}"].rearrange("(t p) f -> p t f", p=128)
    scale = float(HD) ** -0.5
    with Scope(ctx) as S:
        kr = S.ring("kT", 2, [128, L], BF16)
        vr = S.ring("V", 2, [128, nt, 128], BF16)
        qr = S.ring("q", 3, [128, QB], BF16)
        pmr = S.ring("pm", 2, [128, QB], BF16)
        ppr = S.ring("pp", 4, [128, 2, QB], BF16)
        accAr = S.ring("accA", 2, [128, 2, QB], F32)
        accBr = S.ring("accB", 2, [128, 2, QB], F32)
        acsr = S.ring("acs", 2, [128, 2, QB], BF16)
        rcr = S.ring("rc", 2, [128, QB], F32)
        atr = S.ring("at", 2, [128, QB], BF16)
        spr = S.ring("sp", 2, [128, 1024], F32, psum=True)
        otr = S.ring("ot", 2, [128, 512], F32, psum=True)
        rsfr = S.ring("rsf", 2, [128, 512], F32, psum=True)
        pending_fin = []
        pe_pairs = [pr for pr in range(npair) if pr % 3 == 2]

        def run_fin():
            while pending_fin:
                pending_fin.pop(0)()

        for kh in range(NKV):
            kt, ktb = kr.next()
            vt, vtb = vr.next()
            ctx.dma(SP, lambda e: e.dma_start(out=kt[:, NMETA:L], in_=G.scr[f"kT{si}"][kh]), sb=ktb,
                    reads=[G.dbuf[f"kT{si}"]], writes=[ktb])
            ctx.dma(SP, lambda e: e.dma_start(out=kt[:, 0:NMETA], in_=G.scr["kTm"][kh]), sb=ktb,
                    reads=[G.dbuf["kTm"]], writes=[ktb])
            for ta in range(0, nt - 1, 8):
                tb_ = min(nt - 1, ta + 8)
                ctx.dma(SP, lambda e: e.dma_start(out=vt[:, 1 + ta:1 + tb_, :],
                                                  in_=vd[:, ta:tb_, kh * 128:(kh + 1) * 128]), sb=vtb,
                        reads=[G.dbuf[f"v{si}"]], writes=[vtb])
            ctx.dma(SP, lambda e: e.dma_start(out=vt[0:NMETA, 0, :], in_=G.scr["vm"][:, kh * 128:(kh + 1) * 128]),
                    sb=vtb, reads=[G.dbuf["vm"]], writes=[vtb])
            for hq in range(4):
                h = kh * 4 + hq
                for qb in range(n // QB):
                    qt, qtb = qr.next()
                    ctx.dma(SP, lambda e: e.dma_start(out=qt[:], in_=G.scr[f"qT{si}"][h, :, qb * QB:(qb + 1) * QB]),
                            sb=qtb, reads=[G.dbuf[f"qT{si}"]], writes=[qtb])
                    ot, otb = otr.next()
                    rsf, rsfb = rsfr.next()
                    accA, accAb = accAr.next()
                    accB, accBb = accBr.next()
                    ctx.op(POOL, lambda e: e.memset(accA[:], 0.0), writes=[accAb])
                    ctx.op(POOL, lambda e: e.memset(accB[:], 0.0), writes=[accBb])

                    def pv_meta(pm, pmb, ot=ot, otb=otb, accA=accA, accAb=accAb, vt=vt, vtb=vtb):
                        rd = [vtb, pmb]
                        ctx.begin(PE, reads=rd, writes=[otb])
                        ins = nc.tensor.matmul(ot[:, 0:QB], vt[0:NMETA, 0, :], pm[0:NMETA, :], start=True, stop=False)
                        ctx.end(PE, ins, reads=rd, writes=[otb])
                        ctx.op(DVE, lambda e: e.tensor_tensor(out=accA[0:NMETA, 0, :], in0=accA[0:NMETA, 0, :],
                                                              in1=pm[0:NMETA, :], op=ALU.add),
                               reads=[pmb, accAb], writes=[accAb])

                    def pv_pair(pr, pp, ppb, ot=ot, otb=otb, accA=accA, accAb=accAb, accB=accB, accBb=accBb,
                                vt=vt, vtb=vtb, rsf=rsf, rsfb=rsfb):
                        on_pe = pr in pe_pairs
                        rd = [vtb, ppb] + ([cb["ones_b"]] if on_pe else [])
                        wr = [otb] + ([rsfb] if on_pe else [])
                        ctx.begin(PE, reads=rd, writes=wr)
                        nc.tensor.matmul(ot[:, 0:QB], vt[:, 1 + 2 * pr, :], pp[:, 0, :], start=False, stop=False)
                        ins = nc.tensor.matmul(ot[:, 0:QB], vt[:, 2 + 2 * pr, :], pp[:, 1, :], start=False,
                                               stop=(pr == npair - 1))
                        if on_pe:
                            nc.tensor.matmul(rsf[:, 0:QB], c["ones_b"][:, :], pp[:, 0, :], start=(pr == pe_pairs[0]),
                                             stop=False)
                            ins = nc.tensor.matmul(rsf[:, 0:QB], c["ones_b"][:, :], pp[:, 1, :], start=False,
                                                   stop=False)
                        ctx.end(PE, ins, reads=rd, writes=wr)
                        if on_pe:
                            return
                        ac, acb = (accA, accAb) if pr % 2 == 1 else (accB, accBb)
                        ctx.op(DVE, lambda e: e.tensor_tensor(out=ac[:, :, :], in0=ac[:, :, :], in1=pp[:, :, :],
                                                              op=ALU.add),
                               reads=[ppb, acb], writes=[acb])

                    def fin(ot=ot, otb=otb, accA=accA, accAb=accAb, accB=accB, accBb=accBb, h=h, qb=qb,
                            rsf=rsf, rsfb=rsfb):
                        acs, acsb = acsr.next()
                        ctx.op(POOL, lambda e: e.tensor_tensor(out=acs[:, :, :], in0=accA[:, :, :], in1=accB[:, :, :],
                                                               op=ALU.add),
                               reads=[accAb, accBb], writes=[acsb])
                        rd = [acsb, cb["ones_b"]]
                        ctx.begin(PE, reads=rd, writes=[rsfb])
                        nc.tensor.matmul(rsf[:, 0:QB], c["ones_b"][:, :], acs[:, 0, :], start=(len(pe_pairs) == 0),
                                         stop=False)
                        ins = nc.tensor.matmul(rsf[:, 0:QB], c["ones_b"][:, :], acs[:, 1, :], start=False, stop=True)
                        ctx.end(PE, ins, reads=rd, writes=[rsfb])
                        rc, rcb = rcr.next()
                        ctx.op(DVE, lambda e: e.reciprocal(out=rc[:, :], in_=rsf[:, 0:QB]), reads=[rsfb], writes=[rcb])
                        at, atb = atr.next()
                        ctx.op(DVE, lambda e: e.tensor_tensor(out=at[:, :], in0=ot[:, 0:QB], in1=rc[:, :],
                                                              op=ALU.mult),
                               reads=[otb, rcb], writes=[atb])
                        ctx.dma(SP, lambda e: e.dma_start(out=G.scr[f"attnT{si}"][h * 128:(h + 1) * 128,
                                                                                 qb * QB:(qb + 1) * QB],
                                                          in_=at[:, :]),
                                sb=atb, reads=[atb], writes=[G.dbuf[f"attnT{si}"]])

                    sp, spb = spr.next()
                    rd = [ktb, qtb]
                    ctx.begin(PE, reads=rd, writes=[spb])
                    ins = nc.tensor.matmul(sp[0:NMETA, 0:QB], kt[:, 0:NMETA], qt[:, :], start=True, stop=True)
                    ctx.end(PE, ins, reads=rd, writes=[spb])
                    pm, pmb = pmr.next()
                    ctx.op(ACT, lambda e: e.activation(out=pm[0:NMETA, :], in_=sp[0:NMETA, 0:QB], func=AF.Exp,
                                                       scale=scale), reads=[spb], writes=[pmb])
                    pend = (pv_meta, (pm, pmb))
                    for pr in range(npair):
                        c0 = NMETA + pr * 256
                        sp, spb = spr.next()
                        rd = [ktb, qtb]
                        ctx.begin(PE, reads=rd, writes=[spb])
                        nc.tensor.matmul(sp[:, 0:QB], kt[:, c0:c0 + 128], qt[:, :], start=True, stop=True)
                        ins = nc.tensor.matmul(sp[:, 512:512 + QB], kt[:, c0 + 128:c0 + 256], qt[:, :], start=True,
                                               stop=True)
                        ctx.end(PE, ins, reads=rd, writes=[spb])
                        pp, ppb = ppr.next()
                        spv = sp[:, :].rearrange("p (a q) -> p a q", a=2)[:, :, 0:QB]
                        ctx.op(ACT, lambda e: e.activation(out=pp[:, :, :], in_=spv, func=AF.Exp, scale=scale),
                               reads=[spb], writes=[ppb])
                        pend[0](*pend[1])
                        pend = (pv_pair, (pr, pp, ppb))
                        if pr == min(2, npair - 1):
                            run_fin()
                    pend[0](*pend[1])
                    pending_fin.append(fin)
        run_fin()


def phase_G1(G):
    nc, ctx = G.nc, G.ctx
    PE, ACT, DVE, POOL, SP = ctx.PE, ctx.ACT, ctx.DVE, ctx.POOL, ctx.SP
    BW = 256
    w_ao_r = G.wb["w_ao"].rearrange("(c p) n -> p c n", p=128)
    w_fo_r = G.wb["w_fo"].rearrange("(c p) n -> p c n", p=128)
    with Scope(ctx) as S:
        wao, waob = S.sb("wao", [128, 16, D], BF16)
        wfo, wfob = S.sb("wfo", [128, 8, D], BF16)
        for q4 in range(4):
            ctx.dma(SP, lambda e: e.dma_start(out=wao[:, :, q4 * 512:(q4 + 1) * 512],
                                              in_=w_ao_r[:, :, q4 * 512:(q4 + 1) * 512]), sb=waob,
                    reads=[G.wbb[("w_ao", q4)]], writes=[waob])
            ctx.dma(SP, lambda e: e.dma_start(out=wfo[:, :, q4 * 512:(q4 + 1) * 512],
                                              in_=w_fo_r[:, :, q4 * 512:(q4 + 1) * 512]), sb=wfob,
                    reads=[G.wbb[("w_fo", q4)]], writes=[wfob])
        atr = S.ring("at", 2, [128, 16, BW], BF16)
        ztr = S.ring("zt", 2, [128, 8, BW], BF16)
        gar = S.ring("ga", 2, [128, 16, BW], BF16)
        gfr = S.ring("gf", 2, [128, 16, BW], BF16)
        mtr = S.ring("mt", 2, [128, 16, BW], BF16)
        t1r = S.ring("t1", 2, [128, BW], F32)
        t2r = S.ring("t2", 2, [128, BW], F32)
        par = S.ring("pa", 3, [128, 512], F32, psum=True)
        pfr = S.ring("pf", 3, [128, 512], F32, psum=True)
        for si, n in enumerate(G.seq_ns):
            for b0 in range(0, n, BW):
                at, atb = atr.next()
                zt, ztb = ztr.next()
                ga, gab = gar.next()
                gf, gfb_ = gfr.next()
                for (tl, tlb, nm) in ((at, atb, f"attnT{si}"), (zt, ztb, f"ZT{si}"), (ga, gab, f"sga{si}"),
                                      (gf, gfb_, f"sgf{si}")):
                    srcv = G.scr[nm].rearrange("(c p) t -> p c t", p=128)[:, :, b0:b0 + BW]
                    ctx.dma(SP, lambda e: e.dma_start(out=tl[:], in_=srcv), sb=tlb, reads=[G.dbuf[nm]], writes=[tlb])
                mt, mtb = mtr.next()
                for fc in range(16):
                    pa, pab = par.next()
                    rd = [waob, atb]
                    ctx.begin(PE, reads=rd, writes=[pab])
                    for h in range(16):
                        ins = nc.tensor.matmul(pa[:, 0:BW], wao[:, h, fc * 128:(fc + 1) * 128], at[:, h, :],
                                               start=(h == 0), stop=(h == 15))
                    ctx.end(PE, ins, reads=rd, writes=[pab])
                    pf, pfb = pfr.next()
                    rd = [wfob, ztb]
                    ctx.begin(PE, reads=rd, writes=[pfb])
                    for zc in range(8):
                        ins = nc.tensor.matmul(pf[:, 0:BW], wfo[:, zc, fc * 128:(fc + 1) * 128], zt[:, zc, :],
                                               start=(zc == 0), stop=(zc == 7))
                    ctx.end(PE, ins, reads=rd, writes=[pfb])
                    t1, t1b = t1r.next()
                    ctx.op(DVE, lambda e: e.tensor_tensor(out=t1[:, :], in0=pa[:, 0:BW], in1=ga[:, fc, :], op=ALU.mult),
                           reads=[pab, gab], writes=[t1b])
                    t2, t2b = t2r.next()
                    ctx.op(DVE, lambda e: e.tensor_tensor(out=t2[:, :], in0=pf[:, 0:BW], in1=gf[:, fc, :], op=ALU.mult),
                           reads=[pfb, gfb_], writes=[t2b])
                    ctx.op(POOL, lambda e: e.tensor_tensor(out=mt[:, fc, :], in0=t1[:, :], in1=t2[:, :], op=ALU.add),
                           reads=[t1b, t2b], writes=[mtb])
                dstv = G.scr[f"mT{si}"].rearrange("(c p) t -> p c t", p=128)[:, :, b0:b0 + BW]
                ctx.dma(SP, lambda e: e.dma_start(out=dstv, in_=mt[:]), sb=mtb, reads=[mtb],
                        writes=[G.dbuf[f"mT{si}"]])


BIG = 1.0e4


def phase_G2(G):
    nc, ctx = G.nc, G.ctx
    PE, ACT, DVE, POOL, SP = ctx.PE, ctx.ACT, ctx.DVE, ctx.POOL, ctx.SP
    c, cb = G.c, G.cb
    w_out_r = G.wb["w_out"].rearrange("(c p) n -> p c n", p=128)
    w_r_r = G.din["w_r"].rearrange("(c p) n -> p c n", p=128)
    with Scope(ctx) as S:
        wout, woutb = S.sb("wout", [128, 16, D], BF16)
        for q4 in range(4):
            ctx.dma(SP, lambda e: e.dma_start(out=wout[:, :, q4 * 512:(q4 + 1) * 512],
                                              in_=w_out_r[:, :, q4 * 512:(q4 + 1) * 512]), sb=woutb,
                    reads=[G.wbb[("w_out", q4)]], writes=[woutb])
        wr, wrb = S.sb("wr", [128, 16, 36], F32)
        ctx.dma(SP, lambda e: e.dma_start(out=wr[:], in_=w_r_r), sb=wrb, writes=[wrb])
        g2b, g2bb = S.sb("g2b", [128, D], F32)
        ctx.dma(SP, lambda e: e.dma_start(out=g2b[:], in_=G.din["g2b"]), sb=g2bb, writes=[g2bb])
        epsc, epscb = S.sb("epsc", [128, 1], F32)
        ctx.op(DVE, lambda e: e.memset(epsc[:], EPS), writes=[epscb])
        selacc, selaccb = S.sb("selacc", [128, NE], F32)
        ctx.op(DVE, lambda e: e.memset(selacc[:], 0.0), writes=[selaccb])
        mtr = S.ring("mt", 2, [128, 16, 512], BF16)
        xr = S.ring("x", 2, [128, D], F32)
        h1r = S.ring("h1", 2, [128, D], F32)
        junk, junkb = S.sb("junk", [128, D], BF16)
        hnr = S.ring("hn", 3, [128, D], F32)
        hbr = S.ring("hb", 4, [128, D], BF16)
        hT, hTb = S.sb("hT", [128, 16, 128], F32)
        por = S.ring("po", 4, [128, 512], F32, psum=True)
        ptr_ = S.ring("ptr", 2, [128, 512], F32, psum=True)
        plog, plogb = S.ps("plog", [128, 512], F32)
        prank, prankb = S.ps("prank", [128, 512], F32)

        def small(name, w):
            return S.sb(name, [128, w], F32)
        ss, ssb = small("ss", 1)
        sd, sdb = small("sd", 1)
        rs, rsb = small("rs", 1)
        lg, lgb = small("lg", 36)
        gmax, gmaxb = small("gmax", 1)
        ngmax, ngmaxb = small("ngmax", 1)
        gm, gmb = small("gm", 4)
        eg, egb = small("eg", 4)
        se, seb = small("se", 1)
        pg, pgb = small("pg", 1)
        pen, penb = small("pen", 4)
        lm, lmb = small("lm", NE)
        m1, m1b = small("m1", 1)
        mk1, mk1b = small("mk1", NE)
        lm2, lm2b = small("lm2", NE)
        m2, m2b = small("m2", 1)
        mk2, mk2b = small("mk2", NE)
        dd, ddb = small("dd", 1)
        w1, w1b = small("w1", 1)
        w2, w2b = small("w2", 1)
        sel, selb = small("sel", NE)
        rk, rkb = small("rk", NE)
        ov, ovb = small("ov", NE)
        sm, smb = small("sm", NE)
        tm, tmb = small("tm", NE)
        sf1, sf1b = small("sf1", 1)
        sf2, sf2b = small("sf2", 1)

        tiles = []
        tile_g = 0
        for si, n in enumerate(G.seq_ns):
            BW = min(512, n)
            for b0 in range(0, n, BW):
                for tt in range(BW // 128):
                    tiles.append((si, n, BW, b0, tt, b0 + tt * 128, tile_g))
                    tile_g += 1
        st = {}
        cur = {}

        hst = {}

        def dv(fn, reads, writes):
            ctx.op(DVE, fn, reads=reads, writes=writes)

        def sa(ti):
            si, n, BW, b0, tt, r0, tg = tiles[ti]
            if tt == 0:
                mTd = G.scr[f"mT{si}"].rearrange("(c p) t -> p c t", p=128)
                mt, mtb = mtr.next()
                ctx.dma(SP, lambda e: e.dma_start(out=mt[:, :, 0:BW], in_=mTd[:, :, b0:b0 + BW]), sb=mtb,
                        reads=[G.dbuf[f"mT{si}"]], writes=[mtb])
                cur["mt"] = (mt, mtb)
            mt, mtb = cur["mt"]
            xt, xb = xr.next()
            ctx.dma(SP, lambda e: e.dma_start(out=xt[:], in_=G.din[f"x{si}"][r0:r0 + 128, :]), sb=xb,
                    writes=[xb])
            h1, h1b = h1r.next()
            for nb in range(4):
                po, pob = por.next()
                rd = [mtb, woutb]
                ctx.begin(PE, reads=rd, writes=[pob])
                for fc in range(16):
                    ins = nc.tensor.matmul(po[:, :], mt[:, fc, tt * 128:(tt + 1) * 128],
                                           wout[:, fc, nb * 512:(nb + 1) * 512], start=(fc == 0),
                                           stop=(fc == 15))
                ctx.end(PE, ins, reads=rd, writes=[pob])
                ctx.op(DVE, lambda e: e.tensor_tensor(out=h1[:, nb * 512:(nb + 1) * 512], in0=po[:, :],
                                                      in1=xt[:, nb * 512:(nb + 1) * 512], op=ALU.add),
                       reads=[pob, xb], writes=[h1b])
            ctx.dma(SP, lambda e: e.dma_start(out=G.scr[f"h1_{si}"][r0:r0 + 128, :], in_=h1[:]), sb=h1b,
                    reads=[h1b], writes=[G.dbuf[f"h1_{si}"]])
            hst[ti] = (h1, h1b)

        def sb_(ti):
            si, n, BW, b0, tt, r0, tg = tiles[ti]
            h1, h1b = hst.pop(ti)
            ctx.op(ACT, lambda e: e.activation(out=junk[:], in_=h1[:], func=AF.Square, accum_out=ss[:, 0:1]),
                   reads=[h1b], writes=[junkb, ssb])
            ctx.op(ACT, lambda e: e.activation(out=sd[:], in_=ss[:], func=AF.Sqrt, bias=epsc[:, 0:1],
                                               scale=1.0 / D), reads=[ssb, epscb], writes=[sdb])
            ctx.op(DVE, lambda e: e.reciprocal(out=rs[:], in_=sd[:]), reads=[sdb], writes=[rsb])
            hn, hnb = hnr.next()
            ctx.op(DVE, lambda e: e.scalar_tensor_tensor(out=hn[:], in0=h1[:], scalar=rs[:, 0:1], in1=g2b[:],
                                                         op0=ALU.mult, op1=ALU.mult),
                   reads=[h1b, rsb, g2bb], writes=[hnb])
            hb, hbb = hbr.next()
            ctx.op(ACT, lambda e: e.copy(out=hb[:], in_=hn[:]), reads=[hnb], writes=[hbb])
            st[ti] = (hn, hnb, hb, hbb)

        def sc(ti):
            si, n, BW, b0, tt, r0, tg = tiles[ti]
            hn, hnb, hb, hbb = st[ti]
            for q4 in range(4):
                pt, ptb = ptr_.next()
                rd = [hnb, cb["ident_f"]]
                ctx.begin(PE, reads=rd, writes=[ptb])
                for k in range(4):
                    cc = q4 * 4 + k
                    ins = nc.tensor.transpose(out=pt[:, k * 128:(k + 1) * 128],
                                              in_=hn[:, cc * 128:(cc + 1) * 128], identity=c["ident_f"][:, :])
                ctx.end(PE, ins, reads=rd, writes=[ptb])
                ptv = pt[:, :].rearrange("p (c t) -> p c t", c=4)
                ctx.op(ACT, lambda e: e.copy(out=hT[:, q4 * 4:(q4 + 1) * 4, :], in_=ptv), reads=[ptb],
                       writes=[hTb])
            rd = [hTb, wrb]
            ctx.begin(PE, reads=rd, writes=[plogb])
            for cc in range(16):
                ins = nc.tensor.matmul(plog[:, 0:36], hT[:, cc, :], wr[:, cc, :], start=(cc == 0),
                                       stop=(cc == 15))
            ctx.end(PE, ins, reads=rd, writes=[plogb])
        def sd_(ti):
            si, n, BW, b0, tt, r0, tg = tiles[ti]
            dv(lambda e: e.tensor_copy(out=lg[:], in_=plog[:, 0:36]), [plogb], [lgb])
            dv(lambda e: e.reduce_max(out=gmax[:], in_=lg[:, 0:4], axis=AX.X), [lgb], [gmaxb])
            dv(lambda e: e.tensor_scalar(out=gm[:], in0=lg[:, 0:4], scalar1=gmax[:, 0:1], scalar2=None,
                                         op0=ALU.is_equal), [lgb, gmaxb], [gmb])
            dv(lambda e: e.tensor_scalar(out=ngmax[:], in0=gmax[:], scalar1=-1.0, scalar2=None, op0=ALU.mult),
               [gmaxb], [ngmaxb])
            ctx.op(ACT, lambda e: e.activation(out=eg[:], in_=lg[:, 0:4], func=AF.Exp, bias=ngmax[:, 0:1],
                                               scale=1.0, accum_out=se[:, 0:1]),
                   reads=[lgb, ngmaxb], writes=[egb, seb])
            dv(lambda e: e.reciprocal(out=pg[:], in_=se[:]), [seb], [pgb])
            dv(lambda e: e.tensor_scalar(out=pen[:], in0=gm[:], scalar1=BIG, scalar2=-BIG, op0=ALU.mult,
                                         op1=ALU.add), [gmb], [penb])
            for g in range(4):
                dv(lambda e: e.tensor_scalar(out=lm[:, g * 8:(g + 1) * 8], in0=lg[:, 4 + g * 8:4 + (g + 1) * 8],
                                             scalar1=pen[:, g:g + 1], scalar2=None, op0=ALU.add),
                   [lgb, penb], [lmb])
            dv(lambda e: e.reduce_max(out=m1[:], in_=lm[:], axis=AX.X), [lmb], [m1b])
            dv(lambda e: e.tensor_scalar(out=mk1[:], in0=lm[:], scalar1=m1[:, 0:1], scalar2=None,
                                         op0=ALU.is_equal), [lmb, m1b], [mk1b])
            dv(lambda e: e.scalar_tensor_tensor(out=lm2[:], in0=mk1[:], scalar=-BIG, in1=lm[:], op0=ALU.mult,
                                                op1=ALU.add), [mk1b, lmb], [lm2b])
            dv(lambda e: e.reduce_max(out=m2[:], in_=lm2[:], axis=AX.X), [lm2b], [m2b])
            dv(lambda e: e.tensor_scalar(out=mk2[:], in0=lm2[:], scalar1=m2[:, 0:1], scalar2=None,
                                         op0=ALU.is_equal), [lm2b, m2b], [mk2b])
            dv(lambda e: e.tensor_tensor(out=dd[:], in0=m1[:], in1=m2[:], op=ALU.subtract), [m1b, m2b], [ddb])
            ctx.op(ACT, lambda e: e.activation(out=w1[:], in_=dd[:], func=AF.Sigmoid), reads=[ddb], writes=[w1b])
            dv(lambda e: e.tensor_scalar(out=w2[:], in0=w1[:], scalar1=-1.0, scalar2=1.0, op0=ALU.mult,
                                         op1=ALU.add), [w1b], [w2b])
            dv(lambda e: e.tensor_tensor(out=G.cw1[:, tg:tg + 1], in0=pg[:], in1=w1[:], op=ALU.mult),
               [pgb, w1b], [G.cwb])
            dv(lambda e: e.tensor_tensor(out=G.cw2[:, tg:tg + 1], in0=pg[:], in1=w2[:], op=ALU.mult),
               [pgb, w2b], [G.cwb])
            dv(lambda e: e.tensor_tensor(out=sel[:], in0=mk1[:], in1=mk2[:], op=ALU.add), [mk1b, mk2b], [selb])

        def se_(ti):
            si, n, BW, b0, tt, r0, tg = tiles[ti]
            hn, hnb, hb, hbb = st.pop(ti)
            rd = [selb, selaccb, cb["tri_f"], cb["ones_f"]]
            ctx.begin(PE, reads=rd, writes=[prankb])
            nc.tensor.matmul(prank[:, 0:NE], c["tri_f"][:, :], sel[:, :], start=True, stop=False)
            ins = nc.tensor.matmul(prank[:, 0:NE], c["ones_f"][:, :], selacc[:, :], start=False, stop=True)
            ctx.end(PE, ins, reads=rd, writes=[prankb])
            dv(lambda e: e.tensor_copy(out=rk[:], in_=prank[:, 0:NE]), [prankb], [rkb])
            dv(lambda e: e.tensor_tensor(out=selacc[:], in0=selacc[:], in1=sel[:], op=ALU.add),
               [selb, selaccb], [selaccb])
            dv(lambda e: e.tensor_scalar(out=ov[:], in0=rk[:], scalar1=float(CAP), scalar2=1.0e6,
                                         op0=ALU.is_ge, op1=ALU.mult), [rkb], [ovb])
            dv(lambda e: e.tensor_tensor(out=sm[:], in0=rk[:], in1=c["iota_e"][:, :], op=ALU.add),
               [rkb, cb["iota_e"]], [smb])
            dv(lambda e: e.tensor_tensor(out=sm[:], in0=sm[:], in1=ov[:], op=ALU.add), [smb, ovb], [smb])
            dv(lambda e: e.tensor_tensor(out=tm[:], in0=sm[:], in1=mk1[:], op=ALU.mult), [smb, mk1b], [tmb])
            dv(lambda e: e.reduce_sum(out=sf1[:], in_=tm[:], axis=AX.X), [tmb], [sf1b])
            dv(lambda e: e.tensor_tensor(out=tm[:], in0=sm[:], in1=mk2[:], op=ALU.mult), [smb, mk2b], [tmb])
            dv(lambda e: e.reduce_sum(out=sf2[:], in_=tm[:], axis=AX.X), [tmb], [sf2b])
            dv(lambda e: e.tensor_copy(out=G.sl1[:, tg:tg + 1], in_=sf1[:]), [sf1b], [G.slb])
            dv(lambda e: e.tensor_copy(out=G.sl2[:, tg:tg + 1], in_=sf2[:]), [sf2b], [G.slb])
            for slt in (G.sl1, G.sl2):
                ctx.dma(POOL, lambda e: e.indirect_dma_start(
                    out=G.scr["Xs"][:, :], out_offset=bass.IndirectOffsetOnAxis(ap=slt[:, tg:tg + 1], axis=0),
                    in_=hb[:, :], in_offset=None, bounds_check=G.bcreg, oob_is_err=False),
                    sb=hbb, reads=[hbb, G.slb], writes=[G.dbuf["Xs"]])

        NTI = len(tiles)
        for it in range(NTI + 2):
            if 0 <= it - 2 < NTI:
                sd_(it - 2)
            if it < NTI:
                sa(it)
            if 0 <= it - 1 < NTI:
                sc(it - 1)
            if 0 <= it - 2 < NTI:
                se_(it - 2)
            if it < NTI:
                sb_(it)


def phase_M2(G):
    nc, ctx = G.nc, G.ctx
    PE, ACT, DVE, POOL, SP = ctx.PE, ctx.ACT, ctx.DVE, ctx.POOL, ctx.SP
    c, cb = G.c, G.cb
    NST = CAP // 128
    HW = CAP // 2
    with Scope(ctx) as S:
        ur = S.ring("wu", 9, [128, 4096], BF16)
        stgr = S.ring("stg", 3, [128, 4096], F32)
        castk = {"k": 0}

        def load_unit(dst_view, dstb, src_ap, cdim):
            stg, stgb = stgr.next()
            sv = stg[:, :].rearrange("p (c n) -> p c n", c=cdim)
            ctx.dma(SP, lambda e: e.dma_start(out=sv, in_=src_ap), sb=stgb, writes=[stgb])
            k = castk["k"] % 3
            castk["k"] += 1
            if k == 0:
                ctx.op(ACT, lambda e: e.copy(out=dst_view, in_=sv), reads=[stgb], writes=[dstb])
            elif k == 1:
                ctx.op(ACT, lambda e: e.copy(out=dst_view, in_=sv), reads=[stgb], writes=[dstb])
            else:
                ctx.op(DVE, lambda e: e.tensor_copy(out=dst_view, in_=sv), reads=[stgb], writes=[dstb])
        xrr = S.ring("xr", 2, [128, D], BF16)
        xtr = S.ring("XT", 2, [128, 16, CAP], BF16)
        hdr = S.ring("hid", 1, [128, 8, CAP], BF16)
        sgr = S.ring("sg", 2, [128, HW], F32)
        ysr = S.ring("ys", 2, [128, D], F32)
        ptxr = S.ring("ptx", 2, [128, 1024], BF16, psum=True)
        pgr = S.ring("pg", 2, [128, 512], F32, psum=True)
        pur = S.ring("pu", 2, [128, 512], F32, psum=True)
        pyr = S.ring("py", 2, [128, 512], F32, psum=True)
        ulist = []
        for ex_ in range(NE):
            for fp_ in range(4):
                ulist.append((ex_, "g", fp_))
                ulist.append((ex_, "u", fp_))
            for nb_ in range(4):
                ulist.append((ex_, "d", nb_))
        uslots = {}
        ustate = {"issued": 0}
        PF = 5

        def uget(k):
            while ustate["issued"] < min(len(ulist), k + PF + 1):
                kk = ustate["issued"]
                ex_, kind_, idx_ = ulist[kk]
                ut, utb = ur.next()
                if kind_ == "d":
                    view = ut[:, :].rearrange("p (c n) -> p c n", c=8)
                    src = G.din["w_ed"][ex_].rearrange("(c p) f -> p c f", p=128)[:, :, idx_ * 512:(idx_ + 1) * 512]
                    load_unit(view, utb, src, 8)
                else:
                    view = ut[:, :].rearrange("p (c n) -> p c n", c=16)
                    wsrc = G.din["w_eg" if kind_ == "g" else "w_eu"][ex_].rearrange("(c p) f -> p c f", p=128)
                    load_unit(view, utb, wsrc[:, :, idx_ * 256:(idx_ + 1) * 256], 16)
                uslots[kk] = (view, utb)
                ustate["issued"] += 1
            return uslots.pop(k)

        kev = 0
        for ex in range(NE):
            XT, XTb = xtr.next()
            for st in range(NST):
                xrw, xrwb = xrr.next()
                s0 = ex * CAP + st * 128
                ctx.dma(SP, lambda e: e.dma_start(out=xrw[:], in_=G.scr["Xs"][s0:s0 + 128, :]), sb=xrwb,
                        reads=[G.dbuf["Xs"]], writes=[xrwb])
                rd = [xrwb, cb["ident_b"]]
                for hf in range(2):
                    ptx, ptxb = ptxr.next()
                    ctx.begin(PE, reads=rd, writes=[ptxb])
                    for c8 in range(8):
                        cc = hf * 8 + c8
                        ins = nc.tensor.transpose(out=ptx[:, c8 * 128:(c8 + 1) * 128],
                                                  in_=xrw[:, cc * 128:(cc + 1) * 128], identity=c["ident_b"][:, :])
                    ctx.end(PE, ins, reads=rd, writes=[ptxb])
                    ptv = ptx[:, :].rearrange("p (c t) -> p c t", c=8)
                    if kev % 2 == 0:
                        ctx.op(ACT, lambda e: e.copy(out=XT[:, hf * 8:(hf + 1) * 8, st * 128:(st + 1) * 128], in_=ptv),
                               reads=[ptxb], writes=[XTb])
                    else:
                        ctx.op(DVE, lambda e: e.tensor_copy(out=XT[:, hf * 8:(hf + 1) * 8, st * 128:(st + 1) * 128],
                                                            in_=ptv), reads=[ptxb], writes=[XTb])
                    kev += 1
            hid, hidb = hdr.next()
            wg_r = G.din["w_eg"][ex].rearrange("(c p) f -> p c f", p=128)
            wu_r = G.din["w_eu"][ex].rearrange("(c p) f -> p c f", p=128)
            wd_r = G.din["w_ed"][ex].rearrange("(c p) f -> p c f", p=128)
            for fp in range(4):
                ugv, ugb = uget(ex * 12 + fp * 2)
                uuv, uub = uget(ex * 12 + fp * 2 + 1)
                for sub in range(2):
                    fcn = fp * 2 + sub
                    for half in range(2):
                        pg, pgb = pgr.next()
                        pu, pub = pur.next()
                        for (pp, ppb, wv, wvb) in ((pg, pgb, ugv, ugb), (pu, pub, uuv, uub)):
                            rd = [wvb, XTb]
                            ctx.begin(PE, reads=rd, writes=[ppb])
                            for cc in range(16):
                                ins = nc.tensor.matmul(pp[:, 0:HW], wv[:, cc, sub * 128:(sub + 1) * 128],
                                                       XT[:, cc, half * HW:(half + 1) * HW], start=(cc == 0),
                                                       stop=(cc == 15))
                            ctx.end(PE, ins, reads=rd, writes=[ppb])
                        sg, sgb = sgr.next()
                        ctx.op(ACT, lambda e: e.activation(out=sg[:, :], in_=pg[:, 0:HW], func=AF.Silu), reads=[pgb],
                               writes=[sgb])
                        ctx.op(DVE, lambda e: e.tensor_tensor(out=hid[:, fcn, half * HW:(half + 1) * HW], in0=pu[:, 0:HW],
                                                              in1=sg[:, :], op=ALU.mult),
                               reads=[pub, sgb], writes=[hidb])
            uds = []
            for nb in range(4):
                udv, udb = uget(ex * 12 + 8 + nb)
                uds.append((udv, udb))
            for st in range(NST):
                ys, ysb = ysr.next()
                for nb in range(4):
                    udv, udb = uds[nb]
                    py, pyb = pyr.next()
                    rd = [hidb, udb]
                    ctx.begin(PE, reads=rd, writes=[pyb])
                    for fc in range(8):
                        ins = nc.tensor.matmul(py[:, :], hid[:, fc, st * 128:(st + 1) * 128], udv[:, fc, :],
                                               start=(fc == 0), stop=(fc == 7))
                    ctx.end(PE, ins, reads=rd, writes=[pyb])
                    if nb % 2 == 0:
                        ctx.op(ACT, lambda e: e.copy(out=ys[:, nb * 512:(nb + 1) * 512], in_=py[:, :]), reads=[pyb],
                               writes=[ysb])
                    else:
                        ctx.op(DVE, lambda e: e.tensor_copy(out=ys[:, nb * 512:(nb + 1) * 512], in_=py[:, :]),
                               reads=[pyb], writes=[ysb])
                s0 = ex * CAP + st * 128
                ctx.dma(SP, lambda e: e.dma_start(out=G.scr["Ys"][s0:s0 + 128, :], in_=ys[:]), sb=ysb, reads=[ysb],
                        writes=[G.dbuf["Ys"]])


def phase_M3(G):
    nc, ctx = G.nc, G.ctx
    PE, ACT, DVE, POOL, SP = ctx.PE, ctx.ACT, ctx.DVE, ctx.POOL, ctx.SP
    with Scope(ctx) as S:
        gfb, gfbb = S.sb("gfb", [128, D], F32)
        ctx.dma(SP, lambda e: e.dma_start(out=gfb[:], in_=G.din["gfb"]), sb=gfbb, writes=[gfbb])
        epsc, epscb = S.sb("epsc", [128, 1], F32)
        ctx.op(DVE, lambda e: e.memset(epsc[:], EPS), writes=[epscb])
        y1r = S.ring("y1", 4, [128, D], F32)
        y2r = S.ring("y2", 4, [128, D], F32)
        h1r = S.ring("h1", 4, [128, D], F32)
        acr = S.ring("acc", 2, [128, D], F32)
        outr = S.ring("out", 3, [128, D], F32)
        junk, junkb = S.sb("junk", [128, D], BF16)
        ssr = S.ring("ss", 2, [128, 1], F32)
        sdr = S.ring("sd", 2, [128, 1], F32)
        rsr = S.ring("rs", 2, [128, 1], F32)
        tg = 0
        for si, n in enumerate(G.seq_ns):
            for r0 in range(0, n, 128):
                y1, y1b = y1r.next()
                y2, y2b = y2r.next()
                for (yt, ytb, slt) in ((y1, y1b, G.sl1), (y2, y2b, G.sl2)):
                    ctx.dma(POOL, lambda e: e.indirect_dma_start(
                        out=yt[:, :], out_offset=None, in_=G.scr["Ys"][:, :],
                        in_offset=bass.IndirectOffsetOnAxis(ap=slt[:, tg:tg + 1], axis=0),
                        bounds_check=G.bcreg, oob_is_err=False),
                        sb=ytb, reads=[G.dbuf["Ys"], G.slb], writes=[ytb])
                h1, h1b = h1r.next()
                ctx.dma(SP, lambda e: e.dma_start(out=h1[:], in_=G.scr[f"h1_{si}"][r0:r0 + 128, :]), sb=h1b,
                        reads=[G.dbuf[f"h1_{si}"]], writes=[h1b])
                acc, accb = acr.next()
                ctx.op(DVE, lambda e: e.scalar_tensor_tensor(out=acc[:], in0=y1[:], scalar=G.cw1[:, tg:tg + 1], in1=h1[:],
                                                             op0=ALU.mult, op1=ALU.add),
                       reads=[y1b, h1b, G.cwb], writes=[accb])
                ctx.op(DVE, lambda e: e.scalar_tensor_tensor(out=acc[:], in0=y2[:], scalar=G.cw2[:, tg:tg + 1], in1=acc[:],
                                                             op0=ALU.mult, op1=ALU.add),
                       reads=[y2b, accb, G.cwb], writes=[accb])
                ss, ssb = ssr.next()
                ctx.op(ACT, lambda e: e.activation(out=junk[:], in_=acc[:], func=AF.Square, accum_out=ss[:, 0:1]),
                       reads=[accb], writes=[junkb, ssb])
                sd, sdb = sdr.next()
                ctx.op(ACT, lambda e: e.activation(out=sd[:], in_=ss[:], func=AF.Sqrt, bias=epsc[:, 0:1], scale=1.0 / D),
                       reads=[ssb, epscb], writes=[sdb])
                rs, rsb = rsr.next()
                ctx.op(DVE, lambda e: e.reciprocal(out=rs[:], in_=sd[:]), reads=[sdb], writes=[rsb])
                ot, otb = outr.next()
                ctx.op(DVE, lambda e: e.scalar_tensor_tensor(out=ot[:], in0=acc[:], scalar=rs[:, 0:1], in1=gfb[:],
                                                             op0=ALU.mult, op1=ALU.mult),
                       reads=[accb, rsb, gfbb], writes=[otb])
                ctx.dma(SP, lambda e: e.dma_start(out=G.dout[f"y{si}"][r0:r0 + 128, :], in_=ot[:]), sb=otb, reads=[otb],
                        writes=[G.dbuf[f"y{si}"]])
                tg += 1


_PROG_CACHE = {}


def _rep(a):
    return np.ascontiguousarray(np.broadcast_to(np.asarray(a, np.float32).reshape(1, -1), (128, a.size)))


def kernel(x_prompt, x_sample, meta_tokens, mix_norm, w_in, q_gain, k_gain, w_attn_o, w_fourier_o, w_out,
           moe_norm, w_router_group, w_router_expert, w_expert_gate, w_expert_up, w_expert_down, final_norm):
    f = lambda a: np.ascontiguousarray(np.asarray(a, dtype=np.float32))
    x_prompt, x_sample = f(x_prompt), f(x_sample)
    ncores = 8
    seq_ns = [x_prompt.shape[1], x_sample.shape[1], x_sample.shape[1]]
    key = tuple(seq_ns)
    if key not in _PROG_CACHE:
        _PROG_CACHE[key] = build_program(seq_ns)
    nc, G = _PROG_CACHE[key]
    consts = host_consts(seq_ns)
    shared = {
        "meta": f(meta_tokens), "g1b": _rep(f(mix_norm)[0]), "g2b": _rep(f(moe_norm)[0]), "gfb": _rep(f(final_norm)),
        "qg": f(q_gain)[0].reshape(128, 1).copy(), "kg": f(k_gain)[0].reshape(128, 1).copy(),
        "w_in": f(w_in)[0], "w_ao": f(w_attn_o)[0], "w_fo": f(w_fourier_o)[0], "w_out": f(w_out)[0],
        "w_r": np.ascontiguousarray(np.concatenate([f(w_router_group)[0], f(w_router_expert)[0]], axis=1)),
        "w_eg": f(w_expert_gate)[0], "w_eu": f(w_expert_up)[0], "w_ed": f(w_expert_down)[0],
    }
    shared.update(consts)
    in_maps = []
    for cid in range(ncores):
        m = dict(shared)
        m["x0"] = x_prompt[cid]
        m["x1"] = x_sample[2 * cid]
        m["x2"] = x_sample[2 * cid + 1]
        in_maps.append({k: v for k, v in m.items() if k in G.din})
    res = run_bass_kernel_spmd(nc, in_maps, core_ids=list(range(ncores)))
    yp = np.stack([np.asarray(res.results[cid]["y0"], dtype=np.float32) for cid in range(ncores)], axis=0)
    ys = np.stack([np.asarray(res.results[cid][f"y{1 + j}"], dtype=np.float32)
                   for cid in range(ncores) for j in range(2)], axis=0)
    return (yp, ys)
```
